# Optimizing a Trainium2 kernel written in Bass

```python
import jax, jax.numpy as jnp
from jax import lax
import numpy as np

D_MODEL = 1024
BATCH = 16
SEQ = 2048
DEPTH = 1

GRID_W = 64
CTX_LEN = 256
HG_HEADS = 4
HG_DK = 128
HG_DV = 128
HG_KEY = HG_HEADS * HG_DK
HG_VAL = HG_HEADS * HG_DV
HG_CHUNK = 64
ATT_HEADS = 8
ATT_KV_HEADS = 2
ATT_HEAD_DIM = 64
ATT_GROUPS = ATT_HEADS // ATT_KV_HEADS
ATT_Q = ATT_HEADS * ATT_HEAD_DIM
ATT_KV = ATT_KV_HEADS * ATT_HEAD_DIM
Q_BLOCK = 128
ROPE_AXIS_DIM = ATT_HEAD_DIM // 2
ROPE_THETA = 10000.0
N_EXPERTS = 16
EC_CAPACITY_FACTOR = 2
EXPERT_FF = 1024
IN_SPLITS = (HG_KEY, HG_KEY, HG_KEY, HG_VAL, HG_VAL, ATT_Q, ATT_KV, ATT_KV, D_MODEL, D_MODEL)
IN_COLS = 3 * HG_KEY + 2 * HG_VAL + ATT_Q + 2 * ATT_KV + 2 * D_MODEL
N_MOD = 6
NORM_EPS = 1e-6

kernel_name = "hybrid_hgrn2_gqa_ec_moe_diffusion_layer"


def rms_norm(x, g):
    xf = x.astype(jnp.float32)
    y = xf * lax.rsqrt(jnp.mean(xf * xf, axis=-1, keepdims=True) + NORM_EPS)
    return (y * g.astype(jnp.float32)).astype(x.dtype)


def modulate(h, shift, scale):
    return h * (1 + scale) + shift


def adaln_params(cond, w_mod, b_mod):
    m = jax.nn.silu(cond) @ w_mod + b_mod
    return jnp.split(m[..., None, :], N_MOD, axis=-1)


def axial_rope_tables(length):
    rows = length // GRID_W
    row = jnp.repeat(jnp.arange(rows, dtype=jnp.float32), GRID_W)
    col = jnp.tile(jnp.arange(GRID_W, dtype=jnp.float32), rows)
    inv_freq = ROPE_THETA ** (-jnp.arange(0, ROPE_AXIS_DIM, 2, dtype=jnp.float32) / ROPE_AXIS_DIM)
    ang = jnp.concatenate([row[:, None] * inv_freq, col[:, None] * inv_freq], axis=-1)
    return jnp.cos(ang), jnp.sin(ang)


def apply_axial_rope(x, cos, sin):
    b_, length, h_, d = x.shape
    half = ROPE_AXIS_DIM // 2
    xs = x.astype(jnp.float32).reshape(b_, length, h_, 2, ROPE_AXIS_DIM)
    x1, x2 = xs[..., :half], xs[..., half:]
    c = cos.reshape(length, 1, 2, half)
    s = sin.reshape(length, 1, 2, half)
    out = jnp.concatenate([x1 * c - x2 * s, x2 * c + x1 * s], axis=-1)
    return out.reshape(b_, length, h_, d).astype(x.dtype)


def heads(a, n):
    return a.reshape(a.shape[0], a.shape[1], n, -1)


def hgrn_gates(f_pre, lb):
    f_pre = heads(f_pre.astype(jnp.float32), HG_HEADS)
    lb = lb.reshape(HG_HEADS, HG_DK)
    log_f = jnp.log(lb + (1 - lb) * jax.nn.sigmoid(f_pre))
    key = (1 - lb) * jax.nn.sigmoid(-f_pre)
    return key, log_f


def hgrn_chunk_scan(q, k, v, log_f, s0):
    b_, length, h_, _ = q.shape
    dv = v.shape[-1]
    n_chunks = length // HG_CHUNK

    def chunks(a):
        return a.astype(jnp.float32).reshape(b_, n_chunks, HG_CHUNK, h_, a.shape[-1]).transpose(1, 0, 3, 2, 4)

    incl = jnp.tril(jnp.ones((HG_CHUNK, HG_CHUNK), dtype=bool))[:, :, None]

    def step(state, inp):
        qc, kc, vc, fc = inp
        cum = jnp.cumsum(fc, axis=2)
        rel = jnp.where(incl, cum[:, :, :, None, :] - cum[:, :, None, :, :], -jnp.inf)
        scores = jnp.einsum("bhtk,bhsk,bhtsk->bhts", qc, kc, jnp.exp(rel))
        o = (jnp.einsum("bhts,bhsv->bhtv", scores, vc)
             + jnp.einsum("bhtk,bhkv->bhtv", qc * jnp.exp(cum), state))
        last = cum[:, :, -1:, :]
        new_state = (jnp.exp(last[:, :, 0, :, None]) * state
                     + jnp.einsum("bhsk,bhsv->bhkv", kc * jnp.exp(last - cum), vc))
        return new_state, o

    s_fin, o = lax.scan(step, s0.astype(jnp.float32), tuple(chunks(a) for a in (q, k, v, log_f)))
    o = o.transpose(1, 0, 3, 2, 4).reshape(b_, length, h_, dv)
    return o.astype(v.dtype), s_fin


def hgrn_bidir(q, i, k_f, lf_f, k_b, lf_b, s_f0, s_b0):
    o_f, s_f = hgrn_chunk_scan(q, k_f, i, lf_f, s_f0)
    flip = lambda a: a[:, ::-1]
    o_b, s_b = hgrn_chunk_scan(flip(q), flip(k_b), flip(i), flip(lf_b), s_b0)
    return o_f + flip(o_b), s_f, s_b


def attend_blocks(q, k, v):
    b_, lq = q.shape[:2]
    nb = lq // Q_BLOCK
    qb = q.reshape(b_, nb, Q_BLOCK, ATT_KV_HEADS, ATT_GROUPS, ATT_HEAD_DIM).transpose(1, 0, 2, 3, 4, 5)
    scale = ATT_HEAD_DIM ** -0.5

    def one_block(qi):
        s = jnp.einsum("bqhgd,bkhd->bhgqk", qi, k, preferred_element_type=jnp.float32) * scale
        p = jax.nn.softmax(s, axis=-1)
        return jnp.einsum("bhgqk,bkhd->bqhgd", p.astype(v.dtype), v)

    o = lax.map(one_block, qb)
    return o.transpose(1, 0, 2, 3, 4, 5).reshape(b_, lq, ATT_Q)


def token_mix(hx, hc, w_in, lb_f, lb_b, hg_norm_g, q_norm_g, k_norm_g,
              w_branch_a, w_branch_b, w_out, cos, sin, ctx_out):
    split_at = np.cumsum(IN_SPLITS)[:-1].tolist()

    def project(h):
        return jnp.split(h @ w_in, split_at, axis=-1)

    def qk_norm(aq, ak):
        return rms_norm(heads(aq, ATT_HEADS), q_norm_g), rms_norm(heads(ak, ATT_KV_HEADS), k_norm_g)

    def merge(o_hg, g_hg, o_att, ga, gb):
        b_, length = g_hg.shape[:2]
        a = (rms_norm(o_hg, hg_norm_g.reshape(HG_HEADS, HG_DV)).reshape(b_, length, HG_VAL)
             * jax.nn.silu(g_hg)) @ w_branch_a
        bb = o_att @ w_branch_b
        return (jax.nn.sigmoid(ga) * a + jax.nn.sigmoid(gb) * bb) @ w_out

    cq, cff, cfb, ci, cg, caq, cak, cav, cga, cgb = project(hc)
    ckf, clf = hgrn_gates(cff, lb_f)
    ckb, clb = hgrn_gates(cfb, lb_b)
    zeros = jnp.zeros((hc.shape[0], HG_HEADS, HG_DK, HG_DV), jnp.float32)
    co, cs_f, cs_b = hgrn_bidir(heads(cq, HG_HEADS), heads(ci, HG_HEADS), ckf, clf, ckb, clb, zeros, zeros)
    cqa, cka = qk_norm(caq, cak)
    cva = heads(cav, ATT_KV_HEADS)

    xq, xff, xfb, xi, xg, xaq, xak, xav, xga, xgb = project(hx)
    xkf, xlf = hgrn_gates(xff, lb_f)
    xkb, xlb = hgrn_gates(xfb, lb_b)
    xo, _, _ = hgrn_bidir(heads(xq, HG_HEADS), heads(xi, HG_HEADS), xkf, xlf, xkb, xlb, cs_f, cs_b)
    xqa, xka = qk_norm(xaq, xak)
    xqa = apply_axial_rope(xqa, cos, sin)
    xka = apply_axial_rope(xka, cos, sin)
    keys = jnp.concatenate([cka, xka], axis=1)
    vals = jnp.concatenate([cva, heads(xav, ATT_KV_HEADS)], axis=1)
    y_x = merge(xo, xg, attend_blocks(xqa, keys, vals), xga, xgb)
    if not ctx_out:
        return y_x, None
    y_c = merge(co, cg, attend_blocks(cqa, cka, cva), cga, cgb)
    return y_x, y_c


def expert_choice_moe(h, w_router, w_gate, w_up, w_down):
    b_, length, _ = h.shape
    cap = EC_CAPACITY_FACTOR * length // N_EXPERTS
    aff = jax.nn.softmax((h @ w_router).astype(jnp.float32), axis=-1)
    gate, idx = lax.top_k(jnp.swapaxes(aff, 1, 2), cap)
    bidx = jnp.arange(b_)[:, None, None]
    xe = h[bidx, idx]
    hid = jax.nn.silu(jnp.einsum("becd,edf->becf", xe, w_gate)) * jnp.einsum("becd,edf->becf", xe, w_up)
    ye = jnp.einsum("becf,efd->becd", hid, w_down) * gate[..., None].astype(h.dtype)
    return jnp.zeros_like(h).at[bidx, idx].add(ye)


def setup_inputs(seed: int = 0) -> dict:
    key = jax.random.key(seed)
    ks = jax.random.split(key, 24)
    n = jax.random.normal
    f32 = jnp.float32
    d = D_MODEL
    return {
        "x": n(ks[0], (BATCH, SEQ, d), f32),
        "c": n(ks[1], (BATCH, d), f32),
        "ctx": n(ks[2], (BATCH, CTX_LEN, d), f32),
        "c_ctx": n(ks[3], (d,), f32),
        "w_mod": n(ks[4], (DEPTH, d, N_MOD * d), f32) * (0.5 * d ** -0.5),
        "b_mod": n(ks[5], (DEPTH, N_MOD * d), f32) * 0.02,
        "norm1_g": 1.0 + 0.1 * n(ks[6], (DEPTH, d), f32),
        "norm2_g": 1.0 + 0.1 * n(ks[7], (DEPTH, d), f32),
        "w_in": n(ks[8], (DEPTH, d, IN_COLS), f32) * d ** -0.5,
        "hg_lb_logits": 0.5 * n(ks[9], (2, DEPTH + 1, HG_KEY), f32),
        "hg_norm_g": 1.0 + 0.1 * n(ks[10], (DEPTH, HG_VAL), f32),
        "q_norm_g": 1.0 + 0.1 * n(ks[11], (DEPTH, ATT_HEAD_DIM), f32),
        "k_norm_g": 1.0 + 0.1 * n(ks[12], (DEPTH, ATT_HEAD_DIM), f32),
        "w_branch_a": n(ks[13], (DEPTH, HG_VAL, d), f32) * HG_VAL ** -0.5,
        "w_branch_b": n(ks[14], (DEPTH, ATT_Q, d), f32) * ATT_Q ** -0.5,
        "w_out": n(ks[15], (DEPTH, d, d), f32) * d ** -0.5,
        "w_router": n(ks[16], (DEPTH, d, N_EXPERTS), f32) * d ** -0.5,
        "w_exp_gate": n(ks[17], (DEPTH, N_EXPERTS, d, EXPERT_FF), f32) * d ** -0.5,
        "w_exp_up": n(ks[18], (DEPTH, N_EXPERTS, d, EXPERT_FF), f32) * d ** -0.5,
        "w_exp_down": n(ks[19], (DEPTH, N_EXPERTS, EXPERT_FF, d), f32) * EXPERT_FF ** -0.5,
        "final_norm_g": 1.0 + 0.1 * n(ks[20], (d,), f32),
    }


def reference(x, c, ctx, c_ctx, w_mod, b_mod, norm1_g, norm2_g, w_in, hg_lb_logits, hg_norm_g,
              q_norm_g, k_norm_g, w_branch_a, w_branch_b, w_out, w_router, w_exp_gate, w_exp_up,
              w_exp_down, final_norm_g):
    cos, sin = axial_rope_tables(x.shape[1])
    lb_all = jnp.cumsum(jax.nn.softmax(hg_lb_logits.astype(jnp.float32), axis=1), axis=1)
    for l in range(DEPTH):
        last = l == DEPTH - 1
        sh1, sc1, g1, sh2, sc2, g2 = adaln_params(c, w_mod[l], b_mod[l])
        csh1, csc1, cg1, csh2, csc2, cg2 = adaln_params(c_ctx, w_mod[l], b_mod[l])
        hx = modulate(rms_norm(x, norm1_g[l]), sh1, sc1)
        hc = modulate(rms_norm(ctx, norm1_g[l]), csh1, csc1)
        y_x, y_c = token_mix(hx, hc, w_in[l], lb_all[0, l], lb_all[1, l], hg_norm_g[l], q_norm_g[l],
                             k_norm_g[l], w_branch_a[l], w_branch_b[l], w_out[l], cos, sin, not last)
        x = x + g1 * y_x
        x = x + g2 * expert_choice_moe(modulate(rms_norm(x, norm2_g[l]), sh2, sc2),
                                       w_router[l], w_exp_gate[l], w_exp_up[l], w_exp_down[l])
        if not last:
            ctx = ctx + cg1 * y_c
            ctx = ctx + cg2 * expert_choice_moe(modulate(rms_norm(ctx, norm2_g[l]), csh2, csc2),
                                                w_router[l], w_exp_gate[l], w_exp_up[l], w_exp_down[l])
    return rms_norm(x, final_norm_g)
```

```python
import numpy as np
from contextlib import ExitStack
import concourse.bass as bass
import concourse.mybir as mybir
from concourse.bass_utils import run_bass_kernel_spmd

F32 = mybir.dt.float32
BF16 = mybir.dt.bfloat16
AF = mybir.ActivationFunctionType
ALU = mybir.AluOpType
AX = mybir.AxisListType

NCORES = 8
SPC = 2
L = 2048
DM = 1024
CTX = 256
NKEY = CTX + L
EPS = 1e-6
NEXP = 16
CAP = 256
BLK = 512
NBLK = L // BLK
C_Q, C_FF, C_FB, C_I, C_G, C_AQ, C_AK, C_AV, C_GA, C_GB = 0, 512, 1024, 1536, 2048, 2560, 3072, 3200, 3328, 4352

ENG_NAMES = ("pe", "act", "dve", "pool", "sp")
EPOCH = 8000
SAME_ENGINE_SYNC = {"pe": False, "act": True, "dve": True, "pool": True, "sp": True}


class Tile:
    __slots__ = ("name", "h", "last_w", "readers", "psum")

    def __init__(self, name, h, psum=False):
        self.name = name
        self.h = h
        self.last_w = None
        self.readers = []
        self.psum = psum

    def __getitem__(self, idx):
        return self.h[idx]


class Prog:
    def __init__(self, nc, stack):
        self.nc = nc
        self.stack = stack
        self.reg = {}
        self.ins = []
        self.base = 0
        self.dma_count = {}
        self.sems = {}
        self.eng_count = {e: 0 for e in ENG_NAMES}
        self.seen = {e: {} for e in ENG_NAMES}
        self.engs = {"pe": nc.tensor, "act": nc.scalar, "dve": nc.vector, "pool": nc.gpsimd, "sp": nc.sync}
        self.n_wait = 0
        self.uid = 0

    def sb(self, name, shape, dtype, stack=None):
        self.uid += 1
        nm = "%s_%d" % (name, self.uid)
        h = (stack or self.stack).enter_context(self.nc.sbuf_tensor(nm, list(shape), dtype))
        t = Tile(nm, h)
        self.reg[nm] = t
        return t

    def ps(self, name, shape, dtype, stack=None):
        self.uid += 1
        nm = "%s_%d" % (name, self.uid)
        h = (stack or self.stack).enter_context(self.nc.psum_tensor(nm, list(shape), dtype))
        t = Tile(nm, h, psum=True)
        self.reg[nm] = t
        return t

    def pseudo(self, name):
        t = Tile(name, None)
        return t

    def _tiles(self, aps):
        out = []
        for a in aps:
            if a is None or isinstance(a, (int, float)):
                continue
            if isinstance(a, Tile):
                out.append(a)
                continue
            t = self.reg.get(a.tensor.name)
            if t is not None:
                out.append(t)
        return out

    def op(self, engine, fn, outs=(), ins=(), dma_key=None):
        reads = self._tiles(ins)
        writes = self._tiles(outs)
        idx = len(self.ins)
        deps = set()
        for t in reads:
            if t.last_w is not None:
                deps.add(t.last_w)
            if t.psum:
                for r in t.readers:
                    if self.ins[r]["engine"] != engine:
                        deps.add(r)
        for t in writes:
            if t.last_w is not None:
                deps.add(t.last_w)
            for r in t.readers:
                deps.add(r)
        deps.discard(idx)
        if dma_key in ("const", "constc"):
            deps = {d for d in deps if self.ins[d]["dma_key"] != dma_key}
        rec = dict(engine=engine, fn=fn, deps=sorted(d for d in deps if d >= self.base),
                   dma_key=dma_key, signal=False)
        if dma_key is not None:
            self.dma_count[dma_key] = self.dma_count.get(dma_key, 0) + 1
            rec["val"] = 16 * self.dma_count[dma_key]
            rec["sem"] = ("dma", dma_key)
        self.ins.append(rec)
        for t in writes:
            t.last_w = idx
            t.readers = []
        for t in reads:
            if t not in writes:
                t.readers.append(idx)
        return idx

    def dma(self, queue, out, in_, key, xr=(), xw=(), **kw):
        return self.op(queue, lambda e: e.dma_start(out=out, in_=in_, **kw),
                       outs=[out] + list(xw), ins=[in_] + list(xr), dma_key=key)

    def _sem(self, k):
        if k not in self.sems:
            nm = "s_" + "_".join(str(x) for x in k)
            self.sems[k] = self.stack.enter_context(self.nc.semaphore(nm))
        return self.sems[k]

    def _wait(self, ename, d):
        rd = self.ins[d]
        k, v = rd["sem"], rd["val"]
        if self.seen[ename].get(k, 0) >= v:
            return
        self.engs[ename].wait_ge(self._sem(k), v)
        self.n_wait += 1
        self.seen[ename][k] = v

    def flush(self, barrier_engine="sp"):
        ins = self.ins
        lo, hi = self.base, len(ins)
        for i in range(lo, hi):
            r = ins[i]
            for d in r["deps"]:
                rd = ins[d]
                if rd["dma_key"] is not None or rd["engine"] != r["engine"] or SAME_ENGINE_SYNC[r["engine"]]:
                    rd["signal"] = True
        last_of = {}
        lastdma = {}
        for i in range(lo, hi):
            r = ins[i]
            if r["dma_key"] is None:
                last_of[r["engine"]] = i
            else:
                r["signal"] = True
                lastdma[r["dma_key"]] = i
        for e, i in last_of.items():
            ins[i]["signal"] = True
        for i in range(lo, hi):
            r = ins[i]
            if r["dma_key"] in ("const", "constc"):
                r["val"] = 16 * self.dma_count[r["dma_key"]]
        for i in range(lo, hi):
            r = ins[i]
            if r["dma_key"] is None and r["signal"]:
                c = self.eng_count[r["engine"]]
                r["sem"] = ("eng", r["engine"], c // EPOCH)
                r["val"] = c % EPOCH + 1
                self.eng_count[r["engine"]] = c + 1
        for i in range(lo, hi):
            r = ins[i]
            ename = r["engine"]
            for d in r["deps"]:
                rd = ins[d]
                if rd["dma_key"] is None and rd["engine"] == ename and not SAME_ENGINE_SYNC[ename]:
                    continue
                self._wait(ename, d)
            bi = r["fn"](self.engs[ename])
            if r["signal"]:
                bi.then_inc(self._sem(r["sem"]), 16 if r["dma_key"] is not None else 1)
            r["fn"] = None
        for ename in ENG_NAMES:
            for e, i in last_of.items():
                if e != ename:
                    self._wait(ename, i)
            for k, i in lastdma.items():
                self._wait(ename, i)
        self.base = hi
        for t in self.reg.values():
            t.last_w = None
            t.readers = []


def host_consts():
    c = {}
    c["c_ident"] = np.eye(128, dtype=np.float32)
    s = np.arange(128)[:, None]
    t = np.arange(128)[None, :]
    same = (s // 64) == (t // 64)
    c["c_maskF"] = (same & (s <= t)).astype(np.float32)
    c["c_maskB"] = (same & (s >= t)).astype(np.float32)
    r = np.ones((128, 512), np.float32)
    r[:, ::64] = 0.0
    c["c_reset"] = r
    c["c_bones"] = ((s // 64) == (t // 64)).astype(np.float32)
    pm = np.zeros((128, 128), np.float32)
    for m in range(128):
        d = m % 32
        partner = m + 16 if d < 16 else m - 16
        pm[partner, m] = 1.0
    c["c_perm"] = pm
    rot = np.zeros((128, 128), np.float32)
    for m in range(128):
        rot[(m + 64) % 128, m] = 1.0
    c["c_rot64"] = rot
    tt = np.arange(L, dtype=np.float32)
    row = np.floor(tt / 64.0).astype(np.float32)
    col = (tt - row * 64.0).astype(np.float32)
    inv_freq = (np.float32(10000.0) ** (-np.arange(0, 32, 2, dtype=np.float32) / np.float32(32.0))).astype(np.float32)
    C = np.zeros((128, L), np.float32)
    S = np.zeros((128, L), np.float32)
    for p in range(128):
        d = p % 64
        a = d // 32
        j = d % 16
        second = (d % 32) >= 16
        pos = row if a == 0 else col
        ang = (pos * inv_freq[j]).astype(np.float32)
        C[p] = np.cos(ang)
        S[p] = np.sin(ang) * (1.0 if second else -1.0)
    c["c_ropeC"] = C
    c["c_ropeS"] = S
    return c


CONST_SHAPES = {"c_ident": (128, 128), "c_maskF": (128, 128), "c_maskB": (128, 128), "c_reset": (128, 512),
                "c_bones": (128, 128), "c_perm": (128, 128), "c_rot64": (128, 128),
                "c_ropeC": (128, L), "c_ropeS": (128, L)}

IN_SHAPES = {
    "x": (SPC, L, DM), "c": (SPC, DM), "ctx": (SPC, CTX, DM), "c_ctx": (DM,),
    "w_mod": (DM, 6 * DM), "b_mod": (6 * DM,), "norm1_g": (DM,), "norm2_g": (DM,),
    "w_in": (DM, 5376), "hg_lb_logits": (2, 2, 512), "hg_norm_g": (512,), "q_norm_g": (64,), "k_norm_g": (64,),
    "w_branch_a": (512, DM), "w_branch_b": (512, DM), "w_out": (DM, DM), "w_router": (DM, NEXP),
    "w_exp_gate": (NEXP, DM, DM), "w_exp_up": (NEXP, DM, DM), "w_exp_down": (NEXP, DM, DM),
    "final_norm_g": (DM,),
}


class Builder:
    def __init__(self, dbg=None, nsamp=SPC):
        self.dbg = dbg
        self.nsamp = nsamp
        self.nc = bass.Bass("TRN2", target_bir_lowering=False)
        self.D = {}
        self.dbg_items = {}
        self.dbg_col = 0

    def act(self, out, in_, func, **kw):
        ins = [in_] + [v for v in kw.values() if not isinstance(v, (int, float))]
        outs = [out]
        if "accum_out" in kw:
            outs.append(kw["accum_out"])
        self.P.op("act", lambda e: e.activation(out=out, in_=in_, func=func, **kw), outs=outs, ins=ins)

    def tt(self, out, in0, in1, op, eng="dve"):
        self.P.op(eng, lambda e: e.tensor_tensor(out=out, in0=in0, in1=in1, op=op), outs=[out], ins=[in0, in1])

    def ts(self, out, in0, s1, s2, op0, op1=None, eng="dve", accum_out=None):
        kw = {}
        if op1 is not None:
            kw["op1"] = op1
        outs = [out]
        if accum_out is not None:
            kw["accum_out"] = accum_out
            outs.append(accum_out)
        self.P.op(eng, lambda e: e.tensor_scalar(out=out, in0=in0, scalar1=s1, scalar2=s2, op0=op0, **kw),
                  outs=outs, ins=[in0, s1, s2])

    def stt(self, out, in0, scalar, in1, op0, op1):
        self.P.op("dve", lambda e: e.scalar_tensor_tensor(out=out, in0=in0, scalar=scalar, in1=in1, op0=op0, op1=op1),
                  outs=[out], ins=[in0, scalar, in1])

    def cp(self, out, in_, eng="dve"):
        if eng == "act":
            self.P.op("act", lambda e: e.activation(out=out, in_=in_, func=AF.Copy), outs=[out], ins=[in_])
        else:
            self.P.op(eng, lambda e: e.tensor_copy(out=out, in_=in_), outs=[out], ins=[in_])

    def memset(self, ap, val, eng="dve"):
        self.P.op(eng, lambda e: e.memset(ap, val), outs=[ap], ins=[])

    def recip(self, out, in_):
        self.P.op("dve", lambda e: e.reciprocal(out=out, in_=in_), outs=[out], ins=[in_])

    def mm(self, out, lhsT, rhs, start=True, stop=True):
        self.P.op("pe", lambda e: e.matmul(out, lhsT=lhsT, rhs=rhs, start=start, stop=stop),
                  outs=[out], ins=[lhsT, rhs])

    def tr(self, out, in_, ident):
        self.P.op("pe", lambda e: e.transpose(out, in_, ident), outs=[out], ins=[in_, ident])

    def din(self, name, shape, dt=F32):
        self.D[name] = self.nc.dram_tensor(name, list(shape), dt, kind="ExternalInput").ap()
        return self.D[name]

    def dump(self, name, ap, n, parts=128):
        if self.dbg is None:
            return
        if getattr(self, "dbg_st_ph", None) is not self.ph:
            self.dbg_st = self.P.sb("dbgst", [128, 512], F32, self.ph)
            self.dbg_st_ph = self.ph
        st = self.dbg_st
        c0 = self.dbg_col
        self.dbg_items[name] = (c0, n)
        self.dbg_col += n
        for j in range(0, n, 512):
            w = min(512, n - j)
            self.memset(st[:, 0:w], 0.0)
            self.cp(st[0:parts, 0:w], ap[:, j:j + w])
            self.P.dma("sp", self.D["dbg"][:, c0 + j:c0 + j + w], st[:, 0:w], key="dbg")

    def rstd(self, out, in_, mul, tmp):
        self.ts(tmp, in_, mul, EPS, ALU.mult, ALU.add)
        self.act(tmp, tmp, AF.Sqrt)
        self.recip(out, tmp)

    def ring_load(self, src_ap):
        i = self.ring_i % len(self.ring)
        self.ring_i += 1
        slot = self.ring[i]
        shp = src_ap.shape
        if len(shp) == 3 and (shp[1], shp[2]) != (8, 512):
            dst = slot[:].rearrange("p a b -> p (a b)").rearrange("p (a b) -> p a b", b=shp[2])
        else:
            dst = slot[:]
        self.P.dma("pool", dst, src_ap, key="ring%d" % i)
        return slot

    def win_group(self, c0):
        return self.ring_load(self.D["w_in"].rearrange("(kc p) j -> p kc j", p=128)[:, :, c0:c0 + 512])

    def make_gbc(self, s, col0, dst, stack):
        A = self
        P = A.P
        Asb = P.sb("Asb", [128, 8, 128], BF16, stack)
        for kc in range(8):
            A.ts(Asb[:, kc, :], A.ones_b[:], A.scT[:, s, kc:kc + 1], None, ALU.mult)
        P.dma("sp", dst[:], A.D["b_mod"][col0:col0 + DM].partition_broadcast(128), key="gbc_" + dst.name)
        wmod3 = A.D["w_mod"].rearrange("(kc p) j -> p kc j", p=128)
        for half in range(2):
            slot = A.ring_load(wmod3[:, :, col0 + half * 512:col0 + (half + 1) * 512])
            for kc in range(8):
                A.mm(A.pj1[:], Asb[:, kc, :], slot[:, kc, :], start=(kc == 0), stop=(kc == 7))
            A.tt(dst[:, half * 512:(half + 1) * 512], A.pj1[:], dst[:, half * 512:(half + 1) * 512], ALU.add)

    def proj_fm(self, pt, w3, c0, hT, n):
        for kc in range(8):
            self.mm(pt[:, 0:n], w3[:, kc, c0:c0 + 128], hT[kc][:, 0:n], start=(kc == 0), stop=(kc == 7))

    def proj_tm(self, pt_ap, hT, t0, w3, c0, ncols):
        for kc in range(8):
            self.mm(pt_ap, hT[kc][:, t0:t0 + 128], w3[:, kc, c0:c0 + ncols], start=(kc == 0), stop=(kc == 7))

    def norm_to_hT(self, xt, v, hT, col0, scT, shbase):
        A = self
        import os
        NS = int(os.environ.get("NORM_STOP", "9"))
        junk, ss, ss2, rs, xn, ptr = A.junk, A.n_ss, A.n_ss2, A.n_rs, A.xn, A.ptr
        if NS < 1:
            return
        A.act(junk[:, 0:DM], xt[:], AF.Square, accum_out=ss[:])
        if NS < 2:
            return
        A.rstd(rs[:], ss[:], 1.0 / DM, ss2[:])
        if NS < 3:
            return
        A.ts(xn[:], xt[:], rs[:, 0:1], None, ALU.mult)
        if NS < 4:
            return
        for kc in range(8):
            A.tr(ptr[:, kc * 128:(kc + 1) * 128], xn[:, kc * 128:(kc + 1) * 128], A.ident_b[:])
        if NS < 5:
            return
        for kc in range(8):
            o = hT[kc][:, col0:col0 + 128]
            i = ptr[:, kc * 128:(kc + 1) * 128]
            EM = os.environ.get("EVAC_MODE", "mix")
            if (kc % 2 == 0 and EM == "mix") or EM == "act":
                A.act(o, i, AF.Identity, scale=scT[:, kc, v:v + 1], bias=A.modT[:, shbase + kc, v:v + 1])
            else:
                A.ts(o, i, scT[:, kc, v:v + 1], A.modT[:, shbase + kc, v:v + 1], ALU.mult, ALU.add)

    def qknorm(self, pk, n, gain, dest, rope_c0=None):
        A = self
        sq, ms, rr, kn, knb = A.qk_sq, A.qk_ms, A.qk_rr, A.qk_kn, A.qk_knb
        A.act(sq[:, 0:n], pk[:, 0:n], AF.Square)
        A.mm(A.pj1[:, 0:n], A.bones[:], sq[:, 0:n])
        A.ts(ms[:, 0:n], A.pj1[:, 0:n], 1.0 / 64, EPS, ALU.mult, ALU.add)
        A.act(ms[:, 0:n], ms[:, 0:n], AF.Sqrt)
        A.recip(rr[:, 0:n], ms[:, 0:n])
        if rope_c0 is None:
            A.stt(dest, pk[:, 0:n], gain[:, 0:1], rr[:, 0:n], ALU.mult, ALU.mult)
            return
        A.stt(kn[:, 0:n], pk[:, 0:n], gain[:, 0:1], rr[:, 0:n], ALU.mult, ALU.mult)
        A.cp(knb[:, 0:n], kn[:, 0:n], eng="act")
        A.mm(A.pX[:, 0:n], A.perm[:], knb[:, 0:n])
        A.tt(rr[:, 0:n], A.pX[:, 0:n], A.ropeS[:, 0:n], ALU.mult)
        A.tt(kn[:, 0:n], kn[:, 0:n], A.ropeC[:, 0:n], ALU.mult)
        if isinstance(dest, tuple):
            A.tt(dest[0][0:64, 0:n], kn[0:64, 0:n], rr[0:64, 0:n], ALU.add)
            A.tt(dest[1][64:128, 0:n], kn[64:128, 0:n], rr[64:128, 0:n], ALU.add)
        else:
            A.tt(dest, kn[:, 0:n], rr[:, 0:n], ALU.add)

    def gates(self, d, h, pf, pq, n):
        A = self
        sg, lf, key, cum, eP, eM = A.g_sg, A.g_lf, A.g_key, A.g_cum, A.g_eP, A.g_eM
        nch = n // 64
        A.act(sg[:, 0:n], pf[:, 0:n], AF.Sigmoid)
        A.act(lf[:, 0:n], sg[:, 0:n], AF.Ln, scale=A.oml[:, d, h:h + 1], bias=A.lb[:, d, h:h + 1])
        A.ts(key[:, 0:n], sg[:, 0:n], A.noml[:, d, h:h + 1], A.oml[:, d, h:h + 1], ALU.mult, ALU.add)
        A.P.op("dve", lambda e: e.tensor_tensor_scan(out=cum[:, 0:n], data0=A.reset[:, 0:n], data1=lf[:, 0:n],
                                                     initial=0.0, op0=ALU.mult, op1=ALU.add),
               outs=[cum[:, 0:n]], ins=[A.reset[:, 0:n], lf[:, 0:n]])
        cv = cum[:, 0:n].rearrange("p (c k) -> p c k", k=64)
        A.act(A.dtot[h][:, 0:nch], cv[:, :, 63], AF.Exp)
        if d == 0:
            A.tt(lf[:, 0:n].rearrange("p (c k) -> p c k", k=64), cv, cv[:, :, 63:64].to_broadcast([128, nch, 64]),
                 ALU.subtract)
        else:
            A.tt(lf[:, 0:n], cum[:, 0:n], lf[:, 0:n], ALU.subtract)
        A.act(eP[:, 0:n], lf[:, 0:n], AF.Exp)
        A.act(eM[:, 0:n], lf[:, 0:n], AF.Exp, scale=-1.0)
        if d == 0:
            eq, ek = eP, eM
        else:
            eq, ek = eM, eP
        A.tt(A.kt_[h][:, 0:n], key[:, 0:n], ek[:, 0:n], ALU.mult)
        if pq is not None:
            v4 = lambda ap: ap[:, 0:n].rearrange("p (c two k) -> p c two k", two=2, k=64)
            A.tt(v4(A.qm0[h])[:, :, 0, :], v4(pq)[:, :, 0, :], v4(eq)[:, :, 0, :], ALU.mult)
            A.tt(v4(A.qm1[h])[:, :, 1, :], v4(pq)[:, :, 1, :], v4(eq)[:, :, 1, :], ALU.mult)

    def hgrn_tile(self, d, tt, vi_t, S, want_out):
        A = self
        t0 = tt * 128
        mask = A.maskF if d == 0 else A.maskB
        order = (0, 1) if d == 0 else (1, 0)
        for h in range(4):
            hs = slice(h * 128, (h + 1) * 128)
            ktile = A.kt_[h][:, t0:t0 + 128]
            q0 = A.qm0[h][:, t0:t0 + 128]
            q1 = A.qm1[h][:, t0:t0 + 128]
            if want_out:
                A.mm(A.psT[h][:], ktile, q0, start=True, stop=False)
                A.mm(A.psT[h][:], ktile, q1, start=False, stop=True)
                A.tt(A.sTm[h][:], A.psT[h][:], mask[:], ALU.mult)
            A.tr(A.ptk[h % 2][:], ktile, A.ident_b[:])
            A.cp(A.ktT0[h][0:64, :], A.ptk[h % 2][0:64, :], eng="act")
            A.cp(A.ktT1[h][64:128, :], A.ptk[h % 2][64:128, :], eng="dve")
            for cp_ in order:
                rows = slice(cp_ * 64, (cp_ + 1) * 64)
                ci = tt * 2 + cp_
                A.mm(A.pkv[h % 2][cp_][:], (A.ktT0 if cp_ == 0 else A.ktT1)[h][:], vi_t[:, hs])
                if want_out:
                    A.act(A.Shat[h][cp_][:], S[h][:], AF.Copy, scale=A.dtot[h][:, ci:ci + 1])
                A.stt(S[h][:], S[h][:], A.dtot[h][:, ci:ci + 1], A.pkv[h % 2][cp_][:], ALU.mult, ALU.add)
            if want_out:
                A.mm(A.po[:, hs], A.sTm[h][:], vi_t[:, hs], start=True, stop=False)
                A.mm(A.po[:, hs], q0, A.Shat[h][0][:], start=False, stop=False)
                A.mm(A.po[:, hs], q1, A.Shat[h][1][:], start=False, stop=True)

    def build(self):
        nc = self.nc
        A = self
        for k, shp in IN_SHAPES.items():
            if A.dbg is not None and A.dbg != "full1" and k.startswith("w_exp"):
                continue
            A.din(k, shp)
        for k, shp in CONST_SHAPES.items():
            A.din(k, shp)
        out = nc.dram_tensor("out", [SPC, L, DM], F32, kind="ExternalOutput").ap()
        A.D["out"] = out
        if A.dbg is not None:
            A.D["dbg"] = nc.dram_tensor("dbg", [128, 16384], F32, kind="ExternalOutput").ap()
        h2s = nc.dram_tensor("h2s", [SPC, 8, 128, L], BF16, kind="Internal").ap()
        D = A.D
        with ExitStack() as st:
            P = Prog(nc, st)
            A.P = P
            sb, ps = P.sb, P.ps
            A.pj0 = ps("pj0", [128, 512], F32)
            A.pj1 = ps("pj1", [128, 512], F32)
            A.pX = ps("pX", [128, 512], F32)
            A.po = ps("po", [128, 512], F32)
            A.ptr = ps("ptr", [128, 1024], BF16)
            bS = ps("bS", [128, 512], F32)
            bKV = ps("bKV", [128, 512], F32)
            b7 = ps("b7", [128, 512], F32)
            A.bS = bS
            A.bKV = bKV
            A.psT = [bS[:, h * 128:(h + 1) * 128] for h in range(4)]
            A.pkv = [[bKV[:, (h * 2 + c) * 128:(h * 2 + c + 1) * 128] for c in range(2)] for h in range(2)]
            A.pY = b7[:, 0:256]
            A.ptk = [b7[:, 256 + i * 64:256 + (i + 1) * 64].bitcast(BF16) for i in range(2)]

            A.ident_b = sb("ident_b", [128, 128], BF16)
            A.ident_f = sb("ident_f", [128, 128], F32)
            A.maskF = sb("maskF", [128, 128], F32)
            A.maskB = sb("maskB", [128, 128], F32)
            A.reset = sb("reset", [128, 512], F32)
            A.bones = sb("bones", [128, 128], BF16)
            A.perm = sb("perm", [128, 128], BF16)
            A.rot64 = sb("rot64", [128, 128], F32)
            A.ones_b = sb("ones_b", [128, 128], BF16)
            A.modT = sb("modT", [128, 48, 4], F32)
            A.sc1T = sb("sc1T", [128, 8, 3], F32)
            A.sc2T = sb("sc2T", [128, 8, 3], F32)
            A.lb = sb("lb", [128, 2, 4], F32)
            A.oml = sb("oml", [128, 2, 4], F32)
            A.noml = sb("noml", [128, 2, 4], F32)
            A.hgn_bc = sb("hgn_bc", [128, 512], F32)
            A.qng = sb("qng", [128, 1], F32)
            A.kng = sb("kng", [128, 1], F32)
            A.scT = sb("scT", [128, 3, 8], F32)
            A.wk = [sb("wk%d" % i, [128, 8, 128], BF16) for i in range(2)]
            A.wv = sb("wv", [128, 8, 128], BF16)
            A.wr = sb("wr", [128, 8, NEXP], F32)
            A.ring = [sb("ring%d" % i, [128, 8, 512], BF16) for i in range(4)]
            A.ring_i = 0
            A.n_ss = sb("n_ss", [128, 1], F32)
            A.n_ss2 = sb("n_ss2", [128, 1], F32)
            A.n_rs = sb("n_rs", [128, 1], F32)
            A.xn = sb("xn", [128, DM], BF16)
            A.junk = A.xn
            A.wgt = [sb("wgt%d" % s, [128, 16, NEXP], F32) for s in range(SPC)]

            cdma = lambda dst, src, **kw: P.dma("sp", dst, src, key="const", **kw)
            cdma_cast = lambda dst, src, **kw: P.dma("pool", dst, src, key="constc", **kw)

            with ExitStack() as ph:
                A.ph = ph
                cdma_cast(A.ident_b[:], D["c_ident"])
                cdma(A.ident_f[:], D["c_ident"])
                cdma(A.maskF[:], D["c_maskF"])
                cdma(A.maskB[:], D["c_maskB"])
                cdma(A.reset[:], D["c_reset"])
                cdma_cast(A.bones[:], D["c_bones"])
                cdma_cast(A.perm[:], D["c_perm"])
                cdma(A.rot64[:], D["c_rot64"])
                A.memset(A.ones_b[:], 1.0)
                cdma(A.hgn_bc[:], D["hg_norm_g"].partition_broadcast(128))
                for half in range(2):
                    cdma(A.qng[half * 64:(half + 1) * 64, :], D["q_norm_g"].rearrange("(p o) -> p o", o=1))
                    cdma(A.kng[half * 64:(half + 1) * 64, :], D["k_norm_g"].rearrange("(p o) -> p o", o=1))
                w_in3 = D["w_in"].rearrange("(kc p) j -> p kc j", p=128)
                for kvh in range(2):
                    for dup in range(2):
                        cdma_cast(A.wk[kvh][:, :, dup * 64:(dup + 1) * 64],
                                  w_in3[:, :, C_AK + kvh * 64:C_AK + (kvh + 1) * 64])
                cdma_cast(A.wv[:], w_in3[:, :, C_AV:C_AV + 128])
                for kc in range(8):
                    cdma(A.wr[:, kc, :], D["w_router"][kc * 128:(kc + 1) * 128, :])
                vrows = sb("vrows", [128, 128], F32, ph)
                vT = sb("vT", [128, 104], F32, ph)
                A.memset(vrows[:], 0.0)
                cdma(vrows[0:48, :], D["b_mod"].rearrange("(j p) -> j p", p=128))
                cdma(vrows[48:56, :], D["norm1_g"].rearrange("(j p) -> j p", p=128))
                cdma(vrows[56:64, :], D["norm2_g"].rearrange("(j p) -> j p", p=128))
                cdma(vrows[64:80, :], D["hg_lb_logits"].rearrange("d s (h p) -> (d s h) p", p=128))
                for s in range(SPC):
                    cdma(vrows[80 + 8 * s:88 + 8 * s, :], D["c"][s].rearrange("(j p) -> j p", p=128))
                cdma(vrows[96:104, :], D["c_ctx"].rearrange("(j p) -> j p", p=128))
                A.tr(A.pj0[:, 0:128], vrows[:], A.ident_f[:])
                A.cp(vT[:], A.pj0[:, 0:104])
                lg = vT[:, 64:80].rearrange("p (d s h) -> p d s h", d=2, s=2)
                dl = sb("dl", [128, 2, 4], F32, ph)
                A.tt(dl[:], lg[:, :, 0, :], lg[:, :, 1, :], ALU.subtract)
                A.act(A.lb[:], dl[:], AF.Sigmoid)
                A.act(A.oml[:], dl[:], AF.Sigmoid, scale=-1.0)
                A.ts(A.noml[:], A.oml[:], -1.0, None, ALU.mult)
                cT = vT[:, 80:104].rearrange("p (v k) -> p v k", v=3)
                scT = A.scT
                scb = sb("scb", [128, 8, 4], BF16, ph)
                A.act(scT[:], cT, AF.Silu)
                A.memset(scb[:], 0.0)
                for v in range(3):
                    A.cp(scb[:, :, v], scT[:, v, :])
                bmT = vT[:, 0:48].rearrange("p (a o) -> p a o", o=1)
                n1g = vT[:, 48:56].rearrange("p (a o) -> p a o", o=1)
                n2g = vT[:, 56:64].rearrange("p (a o) -> p a o", o=1)
                wmod3 = D["w_mod"].rearrange("(kc p) j -> p kc j", p=128)
                for g in range(12):
                    slot = A.ring_load(wmod3[:, :, g * 512:(g + 1) * 512])
                    for jl in range(4):
                        for kc in range(8):
                            A.mm(A.pj0[:, jl * 4:(jl + 1) * 4], slot[:, kc, jl * 128:(jl + 1) * 128], scb[:, kc, :],
                                 start=(kc == 0), stop=(kc == 7))
                    A.cp(A.modT[:, g * 4:(g + 1) * 4, :].rearrange("p a b -> p (a b)"), A.pj0[:, 0:16])
                A.tt(A.modT[:], A.modT[:], bmT.to_broadcast([128, 48, 4]), ALU.add)
                A.stt(A.sc1T[:], A.modT[:, 8:16, 0:3], 1.0, n1g.to_broadcast([128, 8, 3]), ALU.add, ALU.mult)
                A.stt(A.sc2T[:], A.modT[:, 32:40, 0:3], 1.0, n2g.to_broadcast([128, 8, 3]), ALU.add, ALU.mult)
                if A.dbg == "p0":
                    A.dump("modT", A.modT[:].rearrange("p a b -> p (a b)"), 192)
                    A.dump("sc1T", A.sc1T[:].rearrange("p a b -> p (a b)"), 24)
                    A.dump("lb", A.lb[:].rearrange("p a b -> p (a b)"), 8)
                P.flush()
            if A.dbg == "p0":
                return nc

            for s in range(A.nsamp):
                A.token_mix(s, h2s)
                if A.dbg is not None and A.dbg.startswith("tm"):
                    return nc
                A.moe(s, h2s)
                if A.dbg == "full1":
                    return nc
        return nc

    def token_mix(self, s, h2s):
        A = self
        P = A.P
        D = A.D
        with ExitStack() as ph:
            A.ph = ph
            sb = lambda n, shp, dt: P.sb(n, shp, dt, ph)
            xts = [sb("xt%d" % i, [128, DM], F32) for i in range(2)]
            hT = [sb("hT%d" % k, [128, BLK], BF16) for k in range(8)]
            vi = [sb("vi%d" % i, [128, 512], BF16) for i in range(4)]
            big8 = sb("big8", [128, 2 * DM], F32)
            ytmp = big8[:, 0:DM]
            x1t = big8[:, DM:2 * DM]
            affT = big8[0:NEXP, :]
            tmpf = sb("tmpf", [128, BLK], F32)
            numt = sb("numt", [128, BLK], F32)
            osum = sb("osum", [128, 512], F32)
            u1 = sb("u1", [128, 512], F32)
            A.g_sg = x1t[:, 0:BLK]
            A.g_key = x1t[:, BLK:2 * BLK]
            A.g_eP = ytmp[:, 0:BLK]
            A.g_eM = ytmp[:, BLK:2 * BLK]
            A.g_lf = tmpf[:]
            A.g_cum = numt[:]
            g1bc = sb("g1bc", [128, DM], F32)
            A.kt_ = [sb("kt_%d" % h, [128, BLK], BF16) for h in range(4)]
            A.qm0 = [sb("qm0%d" % h, [128, BLK], BF16) for h in range(4)]
            A.qm1 = [sb("qm1%d" % h, [128, BLK], BF16) for h in range(4)]
            A.dtot = [sb("dtot%d" % h, [128, 8], F32) for h in range(4)]
            A.sTm = [sb("sTm%d" % h, [128, 128], BF16) for h in range(4)]
            A.ktT0 = [sb("ktT0%d" % h, [128, 128], BF16) for h in range(4)]
            A.ktT1 = [sb("ktT1%d" % h, [128, 128], BF16) for h in range(4)]
            A.Shat = [[sb("Shat%d%d" % (h, c), [128, 128], BF16) for c in range(2)] for h in range(4)]
            Sf = [sb("Sf%d" % h, [128, 128], F32) for h in range(4)]
            Sb = [sb("Sb%d" % h, [128, 128], F32) for h in range(4)]
            of = [sb("of%d" % i, [128, 512], BF16) for i in range(16)]
            kdup = [sb("kdup%d" % i, [128, NKEY], BF16) for i in range(2)]
            vext = [sb("vext%d" % i, [128, 2, 192], BF16) for i in range(NKEY // 128)]
            A.ropeC = sb("ropeC", [128, BLK], F32)
            A.ropeS = sb("ropeS", [128, BLK], F32)
            A.qk_sq = sb("qk_sq", [128, BLK], BF16)
            A.qk_ms = osum[:]
            A.qk_rr = u1[:]
            A.qk_kn = x1t[:, 0:BLK]
            A.qk_knb = sb("qk_knb", [128, BLK], BF16)
            sgl = [sb("sgl%d" % i, [128, 512], BF16) for i in range(4)]
            ub = sb("ub", [128, 512], BF16)
            ss4 = sb("ss4", [128, 4], F32)
            ss4b = sb("ss4b", [128, 4], F32)
            rs4 = sb("rs4", [128, 4], F32)
            uT = [sb("uT%d" % k, [128, BLK], BF16) for k in range(4)]
            pT = [sb("pT%d" % i, [128, BLK], BF16) for i in range(3)]
            Dt = sb("Dt", [128, BLK], F32)
            OT = [sb("OT%d" % k, [128, BLK], BF16) for k in range(4)]
            sgA = [sb("sgA%d" % i, [128, BLK], BF16) for i in range(2)]
            mT = [sb("mT%d" % k, [128, BLK], BF16) for k in range(8)]
            h2f = [sb("h2f%d" % k, [128, 128], F32) for k in range(8)]
            h2b = [sb("h2b%d" % k, [128, BLK], BF16) for k in range(8)]
            lgt = sb("lgt", [128, NEXP], F32)
            lmx = sb("lmx", [128, 1], F32)
            lsm = sb("lsm", [128, 1], F32)
            aff = sb("aff", [128, 16, NEXP], F32)
            A.make_gbc(s, 2 * DM, g1bc, ph)
            if A.dbg == "tm_ca":
                A.dump("g1bc", g1bc[:], 1024)
                P.flush()
                return

            xi = [0]

            def load_x(src_ap):
                t = xts[xi[0] % 2]
                xi[0] += 1
                P.dma("sp", t[:], src_ap, key="x_" + t.name)
                return t

            for kt in range(NKEY // 128):
                A.memset(vext[kt][:, :, 64:128], 1.0)
            for h in range(4):
                A.memset(Sf[h][:], 0.0)
                A.memset(Sb[h][:], 0.0)
            A.memset(Dt[:], 1.0)
            for h in range(4):
                A.memset(A.qm0[h][:], 0.0)
                A.memset(A.qm1[h][:], 0.0)
                A.memset(A.ktT0[h][:], 0.0)
                A.memset(A.ktT1[h][:], 0.0)

            if A.dbg == "tm_c0":
                A.dump("g1bc", g1bc[:], 1024)
                P.flush()
                return
            for tt in range(2):
                xt = load_x(D["ctx"][s, tt * 128:(tt + 1) * 128, :])
                A.norm_to_hT(xt, 2, hT, tt * 128, A.sc1T, 0)
            if A.dbg == "tm_c1":
                A.dump("xt", xts[1][:, 0:256], 256)
                A.dump("hT0", hT[0][:, 0:256], 256)
                P.flush()
                return
            n = CTX
            w_ff = A.win_group(C_FF)
            w_fb = A.win_group(C_FB)
            w_i = A.win_group(C_I)
            for tt in range(2):
                A.proj_tm(A.pX[:], hT, tt * 128, w_i, 0, 512)
                A.cp(vi[tt][:], A.pX[:], eng="act")
            for h in range(4):
                A.proj_fm(A.pj0, w_ff, h * 128, hT, n)
                A.gates(0, h, A.pj0, None, n)
            if A.dbg == "tm_c2":
                A.dump("kt0", A.kt_[0][:, 0:256], 256)
                A.dump("vi0", vi[0][:], 512)
                P.flush()
                return
            for tt in range(2):
                A.hgrn_tile(0, tt, vi[tt], Sf, False)
            if A.dbg == "tm_c3":
                A.dump("Sf0", Sf[0][:], 128)
                P.flush()
                return
            for h in range(4):
                A.proj_fm(A.pj0, w_fb, h * 128, hT, n)
                A.gates(1, h, A.pj0, None, n)
            for tt in (1, 0):
                A.hgrn_tile(1, tt, vi[tt], Sb, False)
            for kvh in range(2):
                A.proj_fm(A.pj0, A.wk[kvh], 0, hT, n)
                A.qknorm(A.pj0, n, A.kng, kdup[kvh][:, 0:n], None)
            for tt in range(2):
                A.proj_tm(A.pX[:, 0:128], hT, tt * 128, A.wv, 0, 128)
                for kvh in range(2):
                    A.cp(vext[tt][:, kvh, 0:64], A.pX[:, kvh * 64:(kvh + 1) * 64], eng="act")
                    A.cp(vext[tt][:, kvh, 128:192], A.pX[:, kvh * 64:(kvh + 1) * 64], eng="dve")
            if A.dbg == "tm_ctx":
                A.dump("Sf0", Sf[0][:], 128)
                A.dump("Sb3", Sb[3][:], 128)
                A.dump("kdup1c", kdup[1][:, 0:256], 256)
                A.dump("vext1", vext[1][:].rearrange("p a b -> p (a b)"), 384)
                A.dump("hT0", hT[0][:, 0:256], 256)
                P.flush()
                return

            for b in range(NBLK):
                tok0 = b * BLK
                for tt in range(4):
                    xt = load_x(D["x"][s, tok0 + tt * 128:tok0 + (tt + 1) * 128, :])
                    A.norm_to_hT(xt, s, hT, tt * 128, A.sc1T, 0)
                P.dma("sp", A.ropeC[:], D["c_ropeC"][:, tok0:tok0 + BLK], key="ropeC")
                P.dma("sp", A.ropeS[:], D["c_ropeS"][:, tok0:tok0 + BLK], key="ropeS")
                w_ff = A.win_group(C_FF)
                w_q = A.win_group(C_Q)
                w_i = A.win_group(C_I)
                for h in range(4):
                    A.proj_fm(A.pj0, w_ff, h * 128, hT, BLK)
                    A.proj_fm(A.pj1, w_q, h * 128, hT, BLK)
                    A.gates(0, h, A.pj0, A.pj1, BLK)
                for tt in range(4):
                    A.proj_tm(A.pX[:], hT, tt * 128, w_i, 0, 512)
                    A.cp(vi[tt][:], A.pX[:], eng="act")
                for tt in range(4):
                    A.hgrn_tile(0, tt, vi[tt], Sf, True)
                    A.cp(of[b * 4 + tt][:], A.po[:], eng="act")
                for kvh in range(2):
                    A.proj_fm(A.pj0, A.wk[kvh], 0, hT, BLK)
                    A.qknorm(A.pj0, BLK, A.kng, kdup[kvh][:, CTX + tok0:CTX + tok0 + BLK], 0)
                for tt in range(4):
                    kt = 2 + b * 4 + tt
                    A.proj_tm(A.pX[:, 0:128], hT, tt * 128, A.wv, 0, 128)
                    for kvh in range(2):
                        A.cp(vext[kt][:, kvh, 0:64], A.pX[:, kvh * 64:(kvh + 1) * 64], eng="act")
                        A.cp(vext[kt][:, kvh, 128:192], A.pX[:, kvh * 64:(kvh + 1) * 64], eng="dve")
                if A.dbg == "tm_f0" and b == 0:
                    A.dump("of0", of[0][:], 512)
                    A.dump("of3", of[3][:], 512)
                    A.dump("kdup0", kdup[0][:, CTX:CTX + 512], 512)
                    A.dump("kt0", A.kt_[0][:], 512)
                    A.dump("vi0", vi[0][:], 512)
                    P.flush()
                    return
            if A.dbg == "tm_f":
                A.dump("of15", of[15][:], 512)
                A.dump("Sf1", Sf[1][:], 128)
                P.flush()
                return

            w_in3 = D["w_in"].rearrange("(kc p) j -> p kc j", p=128)
            for b in range(NBLK - 1, -1, -1):
                tok0 = b * BLK
                for tt in range(4):
                    xt = load_x(D["x"][s, tok0 + tt * 128:tok0 + (tt + 1) * 128, :])
                    A.norm_to_hT(xt, s, hT, tt * 128, A.sc1T, 0)
                P.dma("sp", A.ropeC[:], D["c_ropeC"][:, tok0:tok0 + BLK], key="ropeC")
                P.dma("sp", A.ropeS[:], D["c_ropeS"][:, tok0:tok0 + BLK], key="ropeS")
                w_fb = A.win_group(C_FB)
                w_q = A.win_group(C_Q)
                w_i = A.win_group(C_I)
                for h in range(4):
                    A.proj_fm(A.pj0, w_fb, h * 128, hT, BLK)
                    A.proj_fm(A.pj1, w_q, h * 128, hT, BLK)
                    A.gates(1, h, A.pj0, A.pj1, BLK)
                w_g = A.win_group(C_G)
                for tt in range(4):
                    A.proj_tm(A.pX[:], hT, tt * 128, w_i, 0, 512)
                    A.cp(vi[tt][:], A.pX[:], eng="act")
                    A.proj_tm(A.pj0[:], hT, tt * 128, w_g, 0, 512)
                    A.act(sgl[tt][:], A.pj0[:], AF.Silu)
                for tt in (3, 2, 1, 0):
                    A.hgrn_tile(1, tt, vi[tt], Sb, True)
                    gt = b * 4 + tt
                    A.tt(osum[:], A.po[:], of[gt][:], ALU.add)
                    for h in range(4):
                        A.act(A.junk[:, h * 128:(h + 1) * 128], osum[:, h * 128:(h + 1) * 128], AF.Square,
                              accum_out=ss4[:, h:h + 1])
                    A.rstd(rs4[:], ss4[:], 1.0 / 128, ss4b[:])
                    for h in range(4):
                        hs = slice(h * 128, (h + 1) * 128)
                        A.stt(u1[:, hs], osum[:, hs], rs4[:, h:h + 1], A.hgn_bc[:, hs], ALU.mult, ALU.mult)
                    A.tt(ub[:], u1[:], sgl[tt][:], ALU.mult)
                    for kc in range(4):
                        A.tr(A.ptr[:, kc * 128:(kc + 1) * 128], ub[:, kc * 128:(kc + 1) * 128], A.ident_b[:])
                    for kc in range(4):
                        A.cp(uT[kc][:, tt * 128:(tt + 1) * 128], A.ptr[:, kc * 128:(kc + 1) * 128],
                             eng=("act" if kc % 2 == 0 else "dve"))
                if A.dbg == "tm_b3" and b == NBLK - 1:
                    A.dump("uT0", uT[0][:], 512)
                    A.dump("uT3", uT[3][:], 512)
                    P.flush()
                    return
                w_aq = A.win_group(C_AQ)
                for qc in range(4):
                    A.memset(mT[qc][64:128, :], 0.0)
                    A.memset(mT[4 + qc][0:64, :], 0.0)
                    A.proj_fm(A.pj0, w_aq, qc * 128, hT, BLK)
                    A.qknorm(A.pj0, BLK, A.qng, (mT[qc], mT[4 + qc]), 0)
                NKT = NKEY // 128
                seq = [(qc, hh, kt) for qc in range(4) for hh in range(2) for kt in range(NKT)]
                pSs = (A.pj0, A.pj1)
                accs = (A.pX, A.bS)
                fxs = (A.po, A.bKV)

                def emit_S(i):
                    qc_, hh_, kt_ = seq[i]
                    A.mm(pSs[i % 2][:], kdup[qc_ // 2][:, kt_ * 128:(kt_ + 1) * 128], mT[hh_ * 4 + qc_][:])

                emit_S(0)
                for i, (qc, hh, kt) in enumerate(seq):
                    kvh = qc // 2
                    g = i // NKT
                    acc = accs[g % 2]
                    if i + 1 < len(seq):
                        emit_S(i + 1)
                    pt = pT[i % 3]
                    A.act(pt[:], pSs[i % 2][:], AF.Exp, scale=0.125)
                    A.mm(acc[:], vext[kt][:, kvh, hh * 64:hh * 64 + 128], pt[:],
                         start=(kt == 0), stop=(kt == NKT - 1))
                    if kt == NKT - 1:
                        rows = slice(hh * 64, (hh + 1) * 64)
                        den = slice((1 - hh) * 64, (2 - hh) * 64)
                        fx = fxs[g % 2]
                        A.recip(Dt[den, :], acc[den, :])
                        A.mm(fx[:], A.rot64[:], Dt[:])
                        A.cp(numt[rows, :], acc[rows, :], eng="act")
                        A.tt(OT[qc][rows, :], numt[rows, :], fx[rows, :], ALU.mult)
                if A.dbg == "tm_att" and b == NBLK - 1:
                    A.dump("qn0a", mT[0][:], 512)
                    A.dump("qn0b", mT[4][:], 512)
                    A.dump("OT0", OT[0][:], 512)
                    A.dump("OT3", OT[3][:], 512)
                    P.flush()
                    return
                w_a = A.ring_load(D["w_branch_a"].rearrange("(kc p) m -> p kc m", p=128))
                w_a4 = w_a[:].rearrange("p a b -> p (a b)").rearrange("p (a b) -> p a b", b=DM)
                for half in range(2):
                    w_ga = A.win_group(C_GA + half * 512)
                    for j in range(4):
                        dmc = half * 4 + j
                        A.proj_fm(A.pj0, w_ga, j * 128, hT, BLK)
                        sg_ = sgA[dmc % 2]
                        A.act(sg_[:], A.pj0[:], AF.Sigmoid)
                        for kc in range(4):
                            A.mm(A.pj1[:], w_a4[:, kc, dmc * 128:(dmc + 1) * 128], uT[kc][:], start=(kc == 0),
                                 stop=(kc == 3))
                        A.tt(mT[dmc][:], A.pj1[:], sg_[:], ALU.mult)
                w_b = A.ring_load(D["w_branch_b"].rearrange("(kc p) m -> p kc m", p=128))
                w_b4 = w_b[:].rearrange("p a b -> p (a b)").rearrange("p (a b) -> p a b", b=DM)
                for half in range(2):
                    w_gb = A.win_group(C_GB + half * 512)
                    for j in range(4):
                        dmc = half * 4 + j
                        A.proj_fm(A.pj0, w_gb, j * 128, hT, BLK)
                        sg_ = sgA[dmc % 2]
                        A.act(sg_[:], A.pj0[:], AF.Sigmoid)
                        for kc in range(4):
                            A.mm(A.pj1[:], w_b4[:, kc, dmc * 128:(dmc + 1) * 128], OT[kc][:], start=(kc == 0),
                                 stop=(kc == 3))
                        A.tt(tmpf[:], A.pj1[:], sg_[:], ALU.mult)
                        A.tt(mT[dmc][:], tmpf[:], mT[dmc][:], ALU.add)
                w_o = [A.ring_load(D["w_out"].rearrange("(kc p) m -> p kc m", p=128)[:, :, hf * 512:(hf + 1) * 512])
                       for hf in range(2)]
                for tt in range(4):
                    gt = b * 4 + tt
                    xt = load_x(D["x"][s, tok0 + tt * 128:tok0 + (tt + 1) * 128, :])
                    for hf in range(2):
                        pyo = A.pj0 if hf == 0 else A.pj1
                        for kc in range(8):
                            A.mm(pyo[:], mT[kc][:, tt * 128:(tt + 1) * 128], w_o[hf][:, kc, :], start=(kc == 0),
                                 stop=(kc == 7))
                        A.tt(ytmp[:, hf * 512:(hf + 1) * 512], pyo[:], g1bc[:, hf * 512:(hf + 1) * 512], ALU.mult)
                    A.tt(x1t[:], ytmp[:], xt[:], ALU.add)
                    P.dma("sp", D["out"][s, tok0 + tt * 128:tok0 + (tt + 1) * 128, :], x1t[:], key="x1st")
                    A.act(A.junk[:, 0:DM], x1t[:], AF.Square, accum_out=A.n_ss[:])
                    A.rstd(A.n_rs[:], A.n_ss[:], 1.0 / DM, A.n_ss2[:])
                    A.ts(ytmp[:], x1t[:], A.n_rs[:, 0:1], None, ALU.mult)
                    for kc in range(8):
                        pq_ = A.pj0 if kc < 4 else A.pj1
                        A.tr(pq_[:, (kc % 4) * 128:(kc % 4 + 1) * 128], ytmp[:, kc * 128:(kc + 1) * 128], A.ident_f[:])
                    for kc in range(8):
                        pq_ = A.pj0 if kc < 4 else A.pj1
                        src = pq_[:, (kc % 4) * 128:(kc % 4 + 1) * 128]
                        A.act(h2f[kc][:], src, AF.Identity, scale=A.sc2T[:, kc, s:s + 1], bias=A.modT[:, 24 + kc, s:s + 1])
                        A.cp(h2b[kc][:, tt * 128:(tt + 1) * 128], h2f[kc][:])
                    for kc in range(8):
                        A.mm(A.pY[:, 0:NEXP], h2f[kc][:], A.wr[:, kc, :], start=(kc == 0), stop=(kc == 7))
                    A.P.op("dve", lambda e: e.reduce_max(out=lmx[:], in_=A.pY[:, 0:NEXP], axis=AX.X),
                           outs=[lmx[:]], ins=[A.pY[:, 0:NEXP]])
                    A.ts(lmx[:], lmx[:], -1.0, None, ALU.mult)
                    A.act(lgt[:], A.pY[:, 0:NEXP], AF.Exp, bias=lmx[:, 0:1], accum_out=lsm[:])
                    A.recip(lsm[:], lsm[:])
                    A.ts(aff[:, gt, :], lgt[:], lsm[:, 0:1], None, ALU.mult)
                for kc in range(8):
                    P.dma("sp", h2s[s, kc, :, tok0:tok0 + BLK], h2b[kc][:], key="h2st%d" % kc)
                if A.dbg == "tm_b3full" and b == NBLK - 1:
                    A.dump("x1", x1t[:], 1024)
                    A.dump("mT0", mT[0][:], 512)
                    A.dump("aff", aff[:, 15, :], 16)
                    A.dump("h2b0", h2b[0][:], 512)
                    P.flush()
                    return

            for gt in range(16):
                A.tr(A.pY[0:NEXP, 128:256], aff[:, gt, :], A.ident_f[:])
                A.cp(affT[:, gt * 128:(gt + 1) * 128], A.pY[0:NEXP, 128:256])
            lo = sb("lo", [NEXP, 1], F32)
            mid = sb("mid", [NEXP, 1], F32)
            cnt = sb("cnt", [NEXP, 1], F32)
            ge = sb("ge", [NEXP, 1], F32)
            cj = kdup[0][0:NEXP, 0:L]
            A.memset(lo[:], 0.0)
            for k in range(1, 29):
                wk_ = 2.0 ** (-k)
                A.ts(mid[:], lo[:], wk_, None, ALU.add)
                A.ts(cj, affT, mid[:, 0:1], 0.0, ALU.is_ge, ALU.add, accum_out=cnt[:])
                A.ts(ge[:], cnt[:], float(CAP), wk_, ALU.is_ge, ALU.mult)
                A.tt(lo[:], lo[:], ge[:], ALU.add)
            dg = sb("dg", [NEXP, NEXP], F32)
            ones16 = sb("ones16", [NEXP, 128], F32)
            thr = sb("thr", [128, 1, NEXP], F32)
            A.memset(ones16[:], 1.0)
            A.ts(dg[:], A.ident_f[0:NEXP, 0:NEXP], lo[:, 0:1], None, ALU.mult)
            A.mm(A.pY[:, 0:NEXP], ones16[:], dg[:])
            A.cp(thr[:, 0, :], A.pY[:, 0:NEXP])
            msk = sb("msk", [128, 16, NEXP], F32)
            A.tt(msk[:], aff[:], thr[:].to_broadcast([128, 16, NEXP]), ALU.is_ge)
            A.tt(A.wgt[s][:], msk[:], aff[:], ALU.mult)
            if A.dbg == "tm_all":
                A.dump("wgt", A.wgt[s][:].rearrange("p a b -> p (a b)"), 256)
                A.dump("aff", aff[:].rearrange("p a b -> p (a b)"), 256)
            P.flush()

    def moe(self, s, h2s):
        A = self
        P = A.P
        D = A.D
        G = 1024
        with ExitStack() as ph:
            A.ph = ph
            sb = lambda n, shp, dt: P.sb(n, shp, dt, ph)
            ering = [sb("ering%d" % i, [128, 8, DM], BF16) for i in range(4)]
            acc = [sb("acc%d" % i, [128, DM], F32) for i in range(G // 128)]
            h2g = sb("h2g", [128, 8, G], BF16)
            hid = [sb("hid%d" % i, [128, 512], BF16) for i in range(8)]
            sgt = [sb("sgt%d" % i, [128, 512], F32) for i in range(2)]
            x1r = [sb("x1r%d" % i, [128, DM], F32) for i in range(2)]
            g2bc = sb("g2bc", [128, DM], F32)
            fing_bc = sb("fing_bc", [128, DM], F32)
            P.dma("sp", fing_bc[:], D["final_norm_g"].partition_broadcast(128), key="fing")
            A.make_gbc(s, 5 * DM, g2bc, ph)
            eri = [0]

            def eload(src):
                i = eri[0] % len(ering)
                eri[0] += 1
                P.dma("pool", ering[i][:], src.rearrange("(kc p) f -> p kc f", p=128), key="ering%d" % i)
                return ering[i]

            for g in range(L // G):
                tokg = g * G
                P.dma("sp", h2g[:], h2s[s].rearrange("kc p t -> p kc t")[:, :, tokg:tokg + G], key="h2ld")
                for e in range(NEXP):
                    wg = eload(D["w_exp_gate"][e])
                    wu = eload(D["w_exp_up"][e])
                    wd = eload(D["w_exp_down"][e])
                    for blk in range(G // 512):
                        c0 = blk * 512
                        for fc in range(8):
                            for kc in range(8):
                                A.mm(A.pj0[:], wg[:, kc, fc * 128:(fc + 1) * 128], h2g[:, kc, c0:c0 + 512],
                                     start=(kc == 0), stop=(kc == 7))
                            for kc in range(8):
                                A.mm(A.pj1[:], wu[:, kc, fc * 128:(fc + 1) * 128], h2g[:, kc, c0:c0 + 512],
                                     start=(kc == 0), stop=(kc == 7))
                            sg_ = sgt[fc % 2]
                            A.act(sg_[:], A.pj0[:], AF.Silu)
                            A.tt(hid[fc][:], sg_[:], A.pj1[:], ALU.mult)
                        for tt in range(4):
                            ti = blk * 4 + tt
                            gt = g * (G // 128) + ti
                            for hf in range(2):
                                py = A.pX if hf == 0 else A.po
                                for fc in range(8):
                                    A.mm(py[:], hid[fc][:, tt * 128:(tt + 1) * 128], wd[:, fc, hf * 512:(hf + 1) * 512],
                                         start=(fc == 0), stop=(fc == 7))
                                dst = acc[ti][:, hf * 512:(hf + 1) * 512]
                                if e == 0:
                                    A.ts(dst, py[:], A.wgt[s][:, gt, e:e + 1], None, ALU.mult)
                                else:
                                    A.stt(dst, py[:], A.wgt[s][:, gt, e:e + 1], dst, ALU.mult, ALU.add)
                for ti in range(G // 128):
                    gt = g * (G // 128) + ti
                    tok = tokg + ti * 128
                    xr = x1r[ti % 2]
                    P.dma("sp", xr[:], D["out"][s, tok:tok + 128, :], key="x1ld_" + xr.name)
                    fin = acc[ti]
                    A.tt(fin[:], fin[:], g2bc[:], ALU.mult)
                    A.tt(fin[:], fin[:], xr[:], ALU.add)
                    A.act(A.junk[:, 0:DM], fin[:], AF.Square, accum_out=A.n_ss[:])
                    A.rstd(A.n_rs[:], A.n_ss[:], 1.0 / DM, A.n_ss2[:])
                    A.stt(xr[:], fin[:], A.n_rs[:, 0:1], fing_bc[:], ALU.mult, ALU.mult)
                    P.dma("sp", D["out"][s, tok:tok + 128, :], xr[:], key="ost_" + xr.name)
            P.flush()


def _in_maps(inputs):
    consts = host_consts()
    maps = []
    f = lambda a: np.ascontiguousarray(np.asarray(a, dtype=np.float32))
    shared = {
        "c_ctx": f(inputs["c_ctx"]), "w_mod": f(inputs["w_mod"][0]), "b_mod": f(inputs["b_mod"][0]),
        "norm1_g": f(inputs["norm1_g"][0]), "norm2_g": f(inputs["norm2_g"][0]), "w_in": f(inputs["w_in"][0]),
        "hg_lb_logits": f(np.asarray(inputs["hg_lb_logits"])[:, 0:2, :]), "hg_norm_g": f(inputs["hg_norm_g"][0]),
        "q_norm_g": f(inputs["q_norm_g"][0]), "k_norm_g": f(inputs["k_norm_g"][0]),
        "w_branch_a": f(inputs["w_branch_a"][0]), "w_branch_b": f(inputs["w_branch_b"][0]),
        "w_out": f(inputs["w_out"][0]), "w_router": f(inputs["w_router"][0]),
        "w_exp_gate": f(inputs["w_exp_gate"][0]), "w_exp_up": f(inputs["w_exp_up"][0]),
        "w_exp_down": f(inputs["w_exp_down"][0]), "final_norm_g": f(inputs["final_norm_g"]),
    }
    shared.update(consts)
    x = np.asarray(inputs["x"], dtype=np.float32)
    c = np.asarray(inputs["c"], dtype=np.float32)
    ctx = np.asarray(inputs["ctx"], dtype=np.float32)
    for i in range(NCORES):
        m = dict(shared)
        m["x"] = np.ascontiguousarray(x[i * SPC:(i + 1) * SPC])
        m["c"] = np.ascontiguousarray(c[i * SPC:(i + 1) * SPC])
        m["ctx"] = np.ascontiguousarray(ctx[i * SPC:(i + 1) * SPC])
        maps.append(m)
    return maps


def kernel(**inputs):
    maps = _in_maps(inputs)
    nc = Builder().build()
    res = run_bass_kernel_spmd(nc, maps, core_ids=list(range(NCORES)))
    out = np.concatenate([np.asarray(r["out"], dtype=np.float32) for r in res.results], axis=0)
    return out
```

```python
import numpy as np
from contextlib import ExitStack
import concourse.bass as bass
import concourse.mybir as mybir
from concourse.bass_utils import run_bass_kernel_spmd

F32 = mybir.dt.float32
BF16 = mybir.dt.bfloat16
AF = mybir.ActivationFunctionType
ALU = mybir.AluOpType
AX = mybir.AxisListType

NCORES = 8
SPC = 2
L = 2048
DM = 1024
CTX = 256
NKEY = CTX + L
EPS = 1e-6
NEXP = 16
CAP = 256
BLK = 512
NBLK = L // BLK
C_Q, C_FF, C_FB, C_I, C_G, C_AQ, C_AK, C_AV, C_GA, C_GB = 0, 512, 1024, 1536, 2048, 2560, 3072, 3200, 3328, 4352

ENG_NAMES = ("pe", "act", "dve", "pool", "sp")
EPOCH = 8000
SAME_ENGINE_SYNC = {"pe": False, "act": True, "dve": True, "pool": True, "sp": True}


class Tile:
    __slots__ = ("name", "h", "last_w", "readers", "psum")

    def __init__(self, name, h, psum=False):
        self.name = name
        self.h = h
        self.last_w = None
        self.readers = []
        self.psum = psum

    def __getitem__(self, idx):
        return self.h[idx]


class Prog:
    def __init__(self, nc, stack):
        self.nc = nc
        self.stack = stack
        self.reg = {}
        self.ins = []
        self.base = 0
        self.dma_count = {}
        self.sems = {}
        self.eng_count = {e: 0 for e in ENG_NAMES}
        self.seen = {e: {} for e in ENG_NAMES}
        self.engs = {"pe": nc.tensor, "act": nc.scalar, "dve": nc.vector, "pool": nc.gpsimd, "sp": nc.sync}
        self.n_wait = 0
        self.uid = 0

    def sb(self, name, shape, dtype, stack=None):
        self.uid += 1
        nm = "%s_%d" % (name, self.uid)
        h = (stack or self.stack).enter_context(self.nc.sbuf_tensor(nm, list(shape), dtype))
        t = Tile(nm, h)
        self.reg[nm] = t
        return t

    def ps(self, name, shape, dtype, stack=None):
        self.uid += 1
        nm = "%s_%d" % (name, self.uid)
        h = (stack or self.stack).enter_context(self.nc.psum_tensor(nm, list(shape), dtype))
        t = Tile(nm, h, psum=True)
        self.reg[nm] = t
        return t

    def pseudo(self, name):
        t = Tile(name, None)
        return t

    def _tiles(self, aps):
        out = []
        for a in aps:
            if a is None or isinstance(a, (int, float)):
                continue
            if isinstance(a, Tile):
                out.append(a)
                continue
            t = self.reg.get(a.tensor.name)
            if t is not None:
                out.append(t)
        return out

    def op(self, engine, fn, outs=(), ins=(), dma_key=None):
        reads = self._tiles(ins)
        writes = self._tiles(outs)
        idx = len(self.ins)
        deps = set()
        for t in reads:
            if t.last_w is not None:
                deps.add(t.last_w)
            if t.psum:
                for r in t.readers:
                    if self.ins[r]["engine"] != engine:
                        deps.add(r)
        for t in writes:
            if t.last_w is not None:
                deps.add(t.last_w)
            for r in t.readers:
                deps.add(r)
        deps.discard(idx)
        if dma_key in ("const", "constc"):
            deps = {d for d in deps if self.ins[d]["dma_key"] != dma_key}
        rec = dict(engine=engine, fn=fn, deps=sorted(d for d in deps if d >= self.base),
                   dma_key=dma_key, signal=False)
        if dma_key is not None:
            self.dma_count[dma_key] = self.dma_count.get(dma_key, 0) + 1
            rec["val"] = 16 * self.dma_count[dma_key]
            rec["sem"] = ("dma", dma_key)
        self.ins.append(rec)
        for t in writes:
            t.last_w = idx
            t.readers = []
        for t in reads:
            if t not in writes:
                t.readers.append(idx)
        return idx

    def dma(self, queue, out, in_, key, xr=(), xw=(), **kw):
        return self.op(queue, lambda e: e.dma_start(out=out, in_=in_, **kw),
                       outs=[out] + list(xw), ins=[in_] + list(xr), dma_key=key)

    def _sem(self, k):
        if k not in self.sems:
            nm = "s_" + "_".join(str(x) for x in k)
            self.sems[k] = self.stack.enter_context(self.nc.semaphore(nm))
        return self.sems[k]

    def _wait(self, ename, d):
        rd = self.ins[d]
        k, v = rd["sem"], rd["val"]
        if self.seen[ename].get(k, 0) >= v:
            return
        self.engs[ename].wait_ge(self._sem(k), v)
        self.n_wait += 1
        self.seen[ename][k] = v

    def flush(self, barrier_engine="sp"):
        ins = self.ins
        lo, hi = self.base, len(ins)
        for i in range(lo, hi):
            r = ins[i]
            for d in r["deps"]:
                rd = ins[d]
                if rd["dma_key"] is not None or rd["engine"] != r["engine"] or SAME_ENGINE_SYNC[r["engine"]]:
                    rd["signal"] = True
        last_of = {}
        lastdma = {}
        for i in range(lo, hi):
            r = ins[i]
            if r["dma_key"] is None:
                last_of[r["engine"]] = i
            else:
                r["signal"] = True
                lastdma[r["dma_key"]] = i
        for e, i in last_of.items():
            ins[i]["signal"] = True
        for i in range(lo, hi):
            r = ins[i]
            if r["dma_key"] in ("const", "constc"):
                r["val"] = 16 * self.dma_count[r["dma_key"]]
        for i in range(lo, hi):
            r = ins[i]
            if r["dma_key"] is None and r["signal"]:
                c = self.eng_count[r["engine"]]
                r["sem"] = ("eng", r["engine"], c // EPOCH)
                r["val"] = c % EPOCH + 1
                self.eng_count[r["engine"]] = c + 1
        for i in range(lo, hi):
            r = ins[i]
            ename = r["engine"]
            for d in r["deps"]:
                rd = ins[d]
                if rd["dma_key"] is None and rd["engine"] == ename and not SAME_ENGINE_SYNC[ename]:
                    continue
                self._wait(ename, d)
            bi = r["fn"](self.engs[ename])
            if r["signal"]:
                bi.then_inc(self._sem(r["sem"]), 16 if r["dma_key"] is not None else 1)
            r["fn"] = None
        for ename in ENG_NAMES:
            for e, i in last_of.items():
                if e != ename:
                    self._wait(ename, i)
            for k, i in lastdma.items():
                self._wait(ename, i)
        self.base = hi
        for t in self.reg.values():
            t.last_w = None
            t.readers = []


def host_consts():
    c = {}
    c["c_ident"] = np.eye(128, dtype=np.float32)
    s = np.arange(128)[:, None]
    t = np.arange(128)[None, :]
    same = (s // 64) == (t // 64)
    c["c_maskF"] = (same & (s <= t)).astype(np.float32)
    c["c_maskB"] = (same & (s >= t)).astype(np.float32)
    r = np.ones((128, 512), np.float32)
    r[:, ::64] = 0.0
    c["c_reset"] = r
    c["c_bones"] = ((s // 64) == (t // 64)).astype(np.float32)
    pm = np.zeros((128, 128), np.float32)
    for m in range(128):
        d = m % 32
        partner = m + 16 if d < 16 else m - 16
        pm[partner, m] = 1.0
    c["c_perm"] = pm
    rot = np.zeros((128, 128), np.float32)
    for m in range(128):
        rot[(m + 64) % 128, m] = 1.0
    c["c_rot64"] = rot
    tt = np.arange(L, dtype=np.float32)
    row = np.floor(tt / 64.0).astype(np.float32)
    col = (tt - row * 64.0).astype(np.float32)
    inv_freq = (np.float32(10000.0) ** (-np.arange(0, 32, 2, dtype=np.float32) / np.float32(32.0))).astype(np.float32)
    C = np.zeros((128, L), np.float32)
    S = np.zeros((128, L), np.float32)
    for p in range(128):
        d = p % 64
        a = d // 32
        j = d % 16
        second = (d % 32) >= 16
        pos = row if a == 0 else col
        ang = (pos * inv_freq[j]).astype(np.float32)
        C[p] = np.cos(ang)
        S[p] = np.sin(ang) * (1.0 if second else -1.0)
    c["c_ropeC"] = C
    c["c_ropeS"] = S
    c["c_tri"] = (s <= t).astype(np.float32)
    c["c_iota"] = np.tile(np.arange(256, dtype=np.float32)[None, :], (128, 1))
    c["c_iotap"] = np.stack([np.arange(128, dtype=np.float32), np.arange(128, dtype=np.float32) + 128], 1)
    return c


CONST_SHAPES = {"c_ident": (128, 128), "c_maskF": (128, 128), "c_maskB": (128, 128), "c_reset": (128, 512),
                "c_bones": (128, 128), "c_perm": (128, 128), "c_rot64": (128, 128),
                "c_ropeC": (128, L), "c_ropeS": (128, L), "c_tri": (128, 128), "c_iota": (128, 256),
                "c_iotap": (128, 2)}

IN_SHAPES = {
    "x": (SPC, L, DM), "c": (SPC, DM), "ctx": (SPC, CTX, DM), "c_ctx": (DM,),
    "w_mod": (DM, 6 * DM), "b_mod": (6 * DM,), "norm1_g": (DM,), "norm2_g": (DM,),
    "w_in": (DM, 5376), "hg_lb_logits": (2, 2, 512), "hg_norm_g": (512,), "q_norm_g": (64,), "k_norm_g": (64,),
    "w_branch_a": (512, DM), "w_branch_b": (512, DM), "w_out": (DM, DM), "w_router": (DM, NEXP),
    "w_exp_gate": (NEXP, DM, DM), "w_exp_up": (NEXP, DM, DM), "w_exp_down": (NEXP, DM, DM),
    "final_norm_g": (DM,),
}


class Builder:
    def __init__(self, dbg=None, nsamp=SPC):
        self.dbg = dbg
        self.nsamp = nsamp
        self.nc = bass.Bass("TRN2", target_bir_lowering=False)
        self.D = {}
        self.dbg_items = {}
        self.dbg_col = 0

    def act(self, out, in_, func, **kw):
        ins = [in_] + [v for v in kw.values() if not isinstance(v, (int, float))]
        outs = [out]
        if "accum_out" in kw:
            outs.append(kw["accum_out"])
        self.P.op("act", lambda e: e.activation(out=out, in_=in_, func=func, **kw), outs=outs, ins=ins)

    def tt(self, out, in0, in1, op, eng="dve"):
        self.P.op(eng, lambda e: e.tensor_tensor(out=out, in0=in0, in1=in1, op=op), outs=[out], ins=[in0, in1])

    def ts(self, out, in0, s1, s2, op0, op1=None, eng="dve", accum_out=None):
        kw = {}
        if op1 is not None:
            kw["op1"] = op1
        outs = [out]
        if accum_out is not None:
            kw["accum_out"] = accum_out
            outs.append(accum_out)
        self.P.op(eng, lambda e: e.tensor_scalar(out=out, in0=in0, scalar1=s1, scalar2=s2, op0=op0, **kw),
                  outs=outs, ins=[in0, s1, s2])

    def stt(self, out, in0, scalar, in1, op0, op1):
        self.P.op("dve", lambda e: e.scalar_tensor_tensor(out=out, in0=in0, scalar=scalar, in1=in1, op0=op0, op1=op1),
                  outs=[out], ins=[in0, scalar, in1])

    def cp(self, out, in_, eng="dve"):
        if eng == "act":
            self.P.op("act", lambda e: e.activation(out=out, in_=in_, func=AF.Copy), outs=[out], ins=[in_])
        else:
            self.P.op(eng, lambda e: e.tensor_copy(out=out, in_=in_), outs=[out], ins=[in_])

    def memset(self, ap, val, eng="dve"):
        self.P.op(eng, lambda e: e.memset(ap, val), outs=[ap], ins=[])

    def recip(self, out, in_):
        self.P.op("dve", lambda e: e.reciprocal(out=out, in_=in_), outs=[out], ins=[in_])

    def mm(self, out, lhsT, rhs, start=True, stop=True):
        self.P.op("pe", lambda e: e.matmul(out, lhsT=lhsT, rhs=rhs, start=start, stop=stop),
                  outs=[out], ins=[lhsT, rhs])

    def tr(self, out, in_, ident):
        self.P.op("pe", lambda e: e.transpose(out, in_, ident), outs=[out], ins=[in_, ident])

    def din(self, name, shape, dt=F32):
        self.D[name] = self.nc.dram_tensor(name, list(shape), dt, kind="ExternalInput").ap()
        return self.D[name]

    def dump(self, name, ap, n, parts=128):
        if self.dbg is None:
            return
        if getattr(self, "dbg_st_ph", None) is not self.ph:
            self.dbg_st = self.P.sb("dbgst", [128, 512], F32, self.ph)
            self.dbg_st_ph = self.ph
        st = self.dbg_st
        c0 = self.dbg_col
        self.dbg_items[name] = (c0, n)
        self.dbg_col += n
        for j in range(0, n, 512):
            w = min(512, n - j)
            self.memset(st[:, 0:w], 0.0)
            self.cp(st[0:parts, 0:w], ap[:, j:j + w])
            self.P.dma("sp", self.D["dbg"][:, c0 + j:c0 + j + w], st[:, 0:w], key="dbg")

    def rstd(self, out, in_, mul, tmp):
        self.ts(tmp, in_, mul, EPS, ALU.mult, ALU.add)
        self.act(tmp, tmp, AF.Sqrt)
        self.recip(out, tmp)

    def ring_load(self, src_ap):
        i = self.ring_i % len(self.ring)
        self.ring_i += 1
        slot = self.ring[i]
        shp = src_ap.shape
        if len(shp) == 3 and (shp[1], shp[2]) != (8, 512):
            dst = slot[:].rearrange("p a b -> p (a b)").rearrange("p (a b) -> p a b", b=shp[2])
        else:
            dst = slot[:]
        self.P.dma("pool", dst, src_ap, key="ring%d" % i)
        return slot

    def win_group(self, c0):
        return self.ring_load(self.D["w_in"].rearrange("(kc p) j -> p kc j", p=128)[:, :, c0:c0 + 512])

    def make_gbc(self, s, col0, dst, stack, slot_ap=None, asb=None):
        A = self
        P = A.P
        Asb = asb if asb is not None else P.sb("Asb", [128, 8, 128], BF16, stack)
        for kc in range(8):
            A.ts(Asb[:, kc, :], A.ones_b[:], A.scT[:, s, kc:kc + 1], None, ALU.mult)
        P.dma("sp", dst[:], A.D["b_mod"][col0:col0 + DM].partition_broadcast(128), key="gbc_" + dst.name)
        wmod3 = A.D["w_mod"].rearrange("(kc p) j -> p kc j", p=128)
        for half in range(2):
            if slot_ap is None:
                slot = A.ring_load(wmod3[:, :, col0 + half * 512:col0 + (half + 1) * 512])
            else:
                slot = slot_ap
                P.dma("pool", slot, wmod3[:, :, col0 + half * 512:col0 + (half + 1) * 512], key="gbcw")
            for kc in range(8):
                A.mm(A.pj1[:], Asb[:, kc, :], slot[:, kc, :], start=(kc == 0), stop=(kc == 7))
            A.tt(dst[:, half * 512:(half + 1) * 512], A.pj1[:], dst[:, half * 512:(half + 1) * 512], ALU.add)

    def proj_fm(self, pt, w3, c0, hT, n):
        for kc in range(8):
            self.mm(pt[:, 0:n], w3[:, kc, c0:c0 + 128], hT[kc][:, 0:n], start=(kc == 0), stop=(kc == 7))

    def proj_tm(self, pt_ap, hT, t0, w3, c0, ncols):
        for kc in range(8):
            self.mm(pt_ap, hT[kc][:, t0:t0 + 128], w3[:, kc, c0:c0 + ncols], start=(kc == 0), stop=(kc == 7))

    def norm_to_hT(self, xt, v, hT, col0, scT, shbase):
        A = self
        import os
        NS = int(os.environ.get("NORM_STOP", "9"))
        junk, ss, ss2, rs, xn, ptr = A.junk, A.n_ss, A.n_ss2, A.n_rs, A.xn, A.ptr
        if NS < 1:
            return
        A.act(junk[:, 0:DM], xt[:], AF.Square, accum_out=ss[:])
        if NS < 2:
            return
        A.rstd(rs[:], ss[:], 1.0 / DM, ss2[:])
        if NS < 3:
            return
        A.ts(xn[:], xt[:], rs[:, 0:1], None, ALU.mult)
        if NS < 4:
            return
        for kc in range(8):
            A.tr(ptr[:, kc * 128:(kc + 1) * 128], xn[:, kc * 128:(kc + 1) * 128], A.ident_b[:])
        if NS < 5:
            return
        for kc in range(8):
            o = hT[kc][:, col0:col0 + 128]
            i = ptr[:, kc * 128:(kc + 1) * 128]
            EM = os.environ.get("EVAC_MODE", "mix")
            if (kc % 2 == 0 and EM == "mix") or EM == "act":
                A.act(o, i, AF.Identity, scale=scT[:, kc, v:v + 1], bias=A.modT[:, shbase + kc, v:v + 1])
            else:
                A.ts(o, i, scT[:, kc, v:v + 1], A.modT[:, shbase + kc, v:v + 1], ALU.mult, ALU.add)

    def qknorm(self, pk, n, gain, dest, rope_c0=None):
        A = self
        sq, ms, rr, kn, knb = A.qk_sq, A.qk_ms, A.qk_rr, A.qk_kn, A.qk_knb
        A.act(sq[:, 0:n], pk[:, 0:n], AF.Square)
        A.mm(A.pj1[:, 0:n], A.bones[:], sq[:, 0:n])
        A.ts(ms[:, 0:n], A.pj1[:, 0:n], 1.0 / 64, EPS, ALU.mult, ALU.add)
        A.act(ms[:, 0:n], ms[:, 0:n], AF.Sqrt)
        A.recip(rr[:, 0:n], ms[:, 0:n])
        if rope_c0 is None:
            A.stt(dest, pk[:, 0:n], gain[:, 0:1], rr[:, 0:n], ALU.mult, ALU.mult)
            return
        A.stt(kn[:, 0:n], pk[:, 0:n], gain[:, 0:1], rr[:, 0:n], ALU.mult, ALU.mult)
        A.cp(knb[:, 0:n], kn[:, 0:n], eng="act")
        A.mm(A.pX[:, 0:n], A.perm[:], knb[:, 0:n])
        A.tt(rr[:, 0:n], A.pX[:, 0:n], A.ropeS[:, 0:n], ALU.mult)
        A.tt(kn[:, 0:n], kn[:, 0:n], A.ropeC[:, 0:n], ALU.mult)
        if isinstance(dest, tuple):
            A.tt(dest[0][0:64, 0:n], kn[0:64, 0:n], rr[0:64, 0:n], ALU.add)
            A.tt(dest[1][64:128, 0:n], kn[64:128, 0:n], rr[64:128, 0:n], ALU.add)
        else:
            A.tt(dest, kn[:, 0:n], rr[:, 0:n], ALU.add)

    def gates(self, d, h, pf, pq, n):
        A = self
        sg, lf, key, cum, eP, eM = A.g_sg, A.g_lf, A.g_key, A.g_cum, A.g_eP, A.g_eM
        nch = n // 64
        A.act(sg[:, 0:n], pf[:, 0:n], AF.Sigmoid)
        A.act(lf[:, 0:n], sg[:, 0:n], AF.Ln, scale=A.oml[:, d, h:h + 1], bias=A.lb[:, d, h:h + 1])
        A.ts(key[:, 0:n], sg[:, 0:n], A.noml[:, d, h:h + 1], A.oml[:, d, h:h + 1], ALU.mult, ALU.add)
        A.P.op("dve", lambda e: e.tensor_tensor_scan(out=cum[:, 0:n], data0=A.reset[:, 0:n], data1=lf[:, 0:n],
                                                     initial=0.0, op0=ALU.mult, op1=ALU.add),
               outs=[cum[:, 0:n]], ins=[A.reset[:, 0:n], lf[:, 0:n]])
        cv = cum[:, 0:n].rearrange("p (c k) -> p c k", k=64)
        A.act(A.dtot[h][:, 0:nch], cv[:, :, 63], AF.Exp)
        if d == 0:
            A.tt(lf[:, 0:n].rearrange("p (c k) -> p c k", k=64), cv, cv[:, :, 63:64].to_broadcast([128, nch, 64]),
                 ALU.subtract)
        else:
            A.tt(lf[:, 0:n], cum[:, 0:n], lf[:, 0:n], ALU.subtract)
        A.act(eP[:, 0:n], lf[:, 0:n], AF.Exp)
        A.act(eM[:, 0:n], lf[:, 0:n], AF.Exp, scale=-1.0)
        if d == 0:
            eq, ek = eP, eM
        else:
            eq, ek = eM, eP
        A.tt(A.kt_[h][:, 0:n], key[:, 0:n], ek[:, 0:n], ALU.mult)
        if pq is not None:
            v4 = lambda ap: ap[:, 0:n].rearrange("p (c two k) -> p c two k", two=2, k=64)
            A.tt(v4(A.qm0[h])[:, :, 0, :], v4(pq)[:, :, 0, :], v4(eq)[:, :, 0, :], ALU.mult)
            A.tt(v4(A.qm1[h])[:, :, 1, :], v4(pq)[:, :, 1, :], v4(eq)[:, :, 1, :], ALU.mult)

    def hgrn_tile(self, d, tt, vi_t, S, want_out):
        A = self
        t0 = tt * 128
        mask = A.maskF if d == 0 else A.maskB
        order = (0, 1) if d == 0 else (1, 0)
        for h in range(4):
            hs = slice(h * 128, (h + 1) * 128)
            ktile = A.kt_[h][:, t0:t0 + 128]
            q0 = A.qm0[h][:, t0:t0 + 128]
            q1 = A.qm1[h][:, t0:t0 + 128]
            if want_out:
                A.mm(A.psT[h][:], ktile, q0, start=True, stop=False)
                A.mm(A.psT[h][:], ktile, q1, start=False, stop=True)
                A.tt(A.sTm[h][:], A.psT[h][:], mask[:], ALU.mult)
            A.tr(A.ptk[h % 2][:], ktile, A.ident_b[:])
            A.cp(A.ktT0[h][0:64, :], A.ptk[h % 2][0:64, :], eng="act")
            A.cp(A.ktT1[h][64:128, :], A.ptk[h % 2][64:128, :], eng="dve")
            for cp_ in order:
                rows = slice(cp_ * 64, (cp_ + 1) * 64)
                ci = tt * 2 + cp_
                A.mm(A.pkv[h % 2][cp_][:], (A.ktT0 if cp_ == 0 else A.ktT1)[h][:], vi_t[:, hs])
                if want_out:
                    A.act(A.Shat[h][cp_][:], S[h][:], AF.Copy, scale=A.dtot[h][:, ci:ci + 1])
                A.stt(S[h][:], S[h][:], A.dtot[h][:, ci:ci + 1], A.pkv[h % 2][cp_][:], ALU.mult, ALU.add)
            if want_out:
                A.mm(A.po[:, hs], A.sTm[h][:], vi_t[:, hs], start=True, stop=False)
                A.mm(A.po[:, hs], q0, A.Shat[h][0][:], start=False, stop=False)
                A.mm(A.po[:, hs], q1, A.Shat[h][1][:], start=False, stop=True)

    def build(self):
        nc = self.nc
        A = self
        for k, shp in IN_SHAPES.items():
            if A.dbg is not None and A.dbg != "full1" and k.startswith("w_exp"):
                continue
            A.din(k, shp)
        for k, shp in CONST_SHAPES.items():
            A.din(k, shp)
        out = nc.dram_tensor("out", [SPC, L, DM], F32, kind="ExternalOutput").ap()
        A.D["out"] = out
        if A.dbg is not None:
            A.D["dbg"] = nc.dram_tensor("dbg", [128, 16384], F32, kind="ExternalOutput").ap()
        h2s = nc.dram_tensor("xn2s", [SPC, 16, 128, DM], BF16, kind="Internal").ap()
        D = A.D
        with ExitStack() as st:
            P = Prog(nc, st)
            A.P = P
            sb, ps = P.sb, P.ps
            A.pj0 = ps("pj0", [128, 512], F32)
            A.pj1 = ps("pj1", [128, 512], F32)
            A.pX = ps("pX", [128, 512], F32)
            A.po = ps("po", [128, 512], F32)
            A.ptr = ps("ptr", [128, 1024], BF16)
            bS = ps("bS", [128, 512], F32)
            bKV = ps("bKV", [128, 512], F32)
            b7 = ps("b7", [128, 512], F32)
            A.bS = bS
            A.bKV = bKV
            A.psT = [bS[:, h * 128:(h + 1) * 128] for h in range(4)]
            A.pkv = [[bKV[:, (h * 2 + c) * 128:(h * 2 + c + 1) * 128] for c in range(2)] for h in range(2)]
            A.pY = b7[:, 0:256]
            A.ptk = [b7[:, 256 + i * 64:256 + (i + 1) * 64].bitcast(BF16) for i in range(2)]

            A.ident_b = sb("ident_b", [128, 128], BF16)
            A.ident_f = sb("ident_f", [128, 128], F32)
            A.maskF = sb("maskF", [128, 128], F32)
            A.maskB = sb("maskB", [128, 128], F32)
            A.reset = sb("reset", [128, 512], F32)
            A.bones = sb("bones", [128, 128], BF16)
            A.perm = sb("perm", [128, 128], BF16)
            A.rot64 = sb("rot64", [128, 128], F32)
            A.ones_b = sb("ones_b", [128, 128], BF16)
            A.modT = sb("modT", [128, 48, 4], F32)
            A.sc1T = sb("sc1T", [128, 8, 3], F32)
            A.sc2T = sb("sc2T", [128, 8, 3], F32)
            A.lb = sb("lb", [128, 2, 4], F32)
            A.oml = sb("oml", [128, 2, 4], F32)
            A.noml = sb("noml", [128, 2, 4], F32)
            A.hgn_bc = sb("hgn_bc", [128, 512], F32)
            A.qng = sb("qng", [128, 1], F32)
            A.kng = sb("kng", [128, 1], F32)
            A.scT = sb("scT", [128, 3, 8], F32)
            A.wk = [sb("wk%d" % i, [128, 8, 128], BF16) for i in range(2)]
            A.wv = sb("wv", [128, 8, 128], BF16)
            A.wr = sb("wr", [128, 8, NEXP], F32)
            A.ring_i = 0
            A.tri_b = sb("tri_b", [128, 128], BF16)
            A.iota = sb("iota", [128, 256], F32)
            A.iotap = sb("iotap", [128, 2], F32)
            A.n_ss = sb("n_ss", [128, 1], F32)
            A.n_ss2 = sb("n_ss2", [128, 1], F32)
            A.n_rs = sb("n_rs", [128, 1], F32)
            A.xn = sb("xn", [128, DM], BF16)
            A.junk = A.xn
            A.wgt = [sb("wgt%d" % s, [128, 16, NEXP], F32) for s in range(SPC)]

            cdma = lambda dst, src, **kw: P.dma("sp", dst, src, key="const", **kw)
            cdma_cast = lambda dst, src, **kw: P.dma("pool", dst, src, key="constc", **kw)

            with ExitStack() as ph:
                A.ph = ph
                A.ring = [sb("ring%d" % i, [128, 8, 512], BF16, ph) for i in range(4)]
                cdma_cast(A.tri_b[:], D["c_tri"])
                cdma(A.iota[:], D["c_iota"])
                cdma(A.iotap[:], D["c_iotap"])
                cdma_cast(A.ident_b[:], D["c_ident"])
                cdma(A.ident_f[:], D["c_ident"])
                cdma(A.maskF[:], D["c_maskF"])
                cdma(A.maskB[:], D["c_maskB"])
                cdma(A.reset[:], D["c_reset"])
                cdma_cast(A.bones[:], D["c_bones"])
                cdma_cast(A.perm[:], D["c_perm"])
                cdma(A.rot64[:], D["c_rot64"])
                A.memset(A.ones_b[:], 1.0)
                cdma(A.hgn_bc[:], D["hg_norm_g"].partition_broadcast(128))
                for half in range(2):
                    cdma(A.qng[half * 64:(half + 1) * 64, :], D["q_norm_g"].rearrange("(p o) -> p o", o=1))
                    cdma(A.kng[half * 64:(half + 1) * 64, :], D["k_norm_g"].rearrange("(p o) -> p o", o=1))
                w_in3 = D["w_in"].rearrange("(kc p) j -> p kc j", p=128)
                for kvh in range(2):
                    for dup in range(2):
                        cdma_cast(A.wk[kvh][:, :, dup * 64:(dup + 1) * 64],
                                  w_in3[:, :, C_AK + kvh * 64:C_AK + (kvh + 1) * 64])
                cdma_cast(A.wv[:], w_in3[:, :, C_AV:C_AV + 128])
                for kc in range(8):
                    cdma(A.wr[:, kc, :], D["w_router"][kc * 128:(kc + 1) * 128, :])
                vrows = sb("vrows", [128, 128], F32, ph)
                vT = sb("vT", [128, 104], F32, ph)
                A.memset(vrows[:], 0.0)
                cdma(vrows[0:48, :], D["b_mod"].rearrange("(j p) -> j p", p=128))
                cdma(vrows[48:56, :], D["norm1_g"].rearrange("(j p) -> j p", p=128))
                cdma(vrows[56:64, :], D["norm2_g"].rearrange("(j p) -> j p", p=128))
                cdma(vrows[64:80, :], D["hg_lb_logits"].rearrange("d s (h p) -> (d s h) p", p=128))
                for s in range(SPC):
                    cdma(vrows[80 + 8 * s:88 + 8 * s, :], D["c"][s].rearrange("(j p) -> j p", p=128))
                cdma(vrows[96:104, :], D["c_ctx"].rearrange("(j p) -> j p", p=128))
                A.tr(A.pj0[:, 0:128], vrows[:], A.ident_f[:])
                A.cp(vT[:], A.pj0[:, 0:104])
                lg = vT[:, 64:80].rearrange("p (d s h) -> p d s h", d=2, s=2)
                dl = sb("dl", [128, 2, 4], F32, ph)
                A.tt(dl[:], lg[:, :, 0, :], lg[:, :, 1, :], ALU.subtract)
                A.act(A.lb[:], dl[:], AF.Sigmoid)
                A.act(A.oml[:], dl[:], AF.Sigmoid, scale=-1.0)
                A.ts(A.noml[:], A.oml[:], -1.0, None, ALU.mult)
                cT = vT[:, 80:104].rearrange("p (v k) -> p v k", v=3)
                scT = A.scT
                scb = sb("scb", [128, 8, 4], BF16, ph)
                A.act(scT[:], cT, AF.Silu)
                A.memset(scb[:], 0.0)
                for v in range(3):
                    A.cp(scb[:, :, v], scT[:, v, :])
                bmT = vT[:, 0:48].rearrange("p (a o) -> p a o", o=1)
                n1g = vT[:, 48:56].rearrange("p (a o) -> p a o", o=1)
                n2g = vT[:, 56:64].rearrange("p (a o) -> p a o", o=1)
                wmod3 = D["w_mod"].rearrange("(kc p) j -> p kc j", p=128)
                for g in range(12):
                    slot = A.ring_load(wmod3[:, :, g * 512:(g + 1) * 512])
                    for jl in range(4):
                        for kc in range(8):
                            A.mm(A.pj0[:, jl * 4:(jl + 1) * 4], slot[:, kc, jl * 128:(jl + 1) * 128], scb[:, kc, :],
                                 start=(kc == 0), stop=(kc == 7))
                    A.cp(A.modT[:, g * 4:(g + 1) * 4, :].rearrange("p a b -> p (a b)"), A.pj0[:, 0:16])
                A.tt(A.modT[:], A.modT[:], bmT.to_broadcast([128, 48, 4]), ALU.add)
                A.stt(A.sc1T[:], A.modT[:, 8:16, 0:3], 1.0, n1g.to_broadcast([128, 8, 3]), ALU.add, ALU.mult)
                A.stt(A.sc2T[:], A.modT[:, 32:40, 0:3], 1.0, n2g.to_broadcast([128, 8, 3]), ALU.add, ALU.mult)
                if A.dbg == "p0":
                    A.dump("modT", A.modT[:].rearrange("p a b -> p (a b)"), 192)
                    A.dump("sc1T", A.sc1T[:].rearrange("p a b -> p (a b)"), 24)
                    A.dump("lb", A.lb[:].rearrange("p a b -> p (a b)"), 8)
                P.flush()
            if A.dbg == "p0":
                return nc

            for s in range(A.nsamp):
                A.token_mix(s, h2s)
                if A.dbg is not None and A.dbg.startswith("tm"):
                    return nc
                A.moe(s, h2s)
                if A.dbg == "full1":
                    return nc
        return nc

    def token_mix(self, s, h2s):
        A = self
        P = A.P
        D = A.D
        with ExitStack() as ph:
            A.ph = ph
            sb = lambda n, shp, dt: P.sb(n, shp, dt, ph)
            A.ring = [sb("ring%d" % i, [128, 8, 512], BF16) for i in range(4)]
            xts = [sb("xt%d" % i, [128, DM], F32) for i in range(2)]
            hT = [sb("hT%d" % k, [128, BLK], BF16) for k in range(8)]
            vi = [sb("vi%d" % i, [128, 512], BF16) for i in range(4)]
            big8 = sb("big8", [128, 2 * DM], F32)
            ytmp = big8[:, 0:DM]
            x1t = big8[:, DM:2 * DM]
            affT = big8[0:NEXP, :]
            tmpf = sb("tmpf", [128, BLK], F32)
            numt = sb("numt", [128, BLK], F32)
            osum = sb("osum", [128, 512], F32)
            u1 = sb("u1", [128, 512], F32)
            A.g_sg = x1t[:, 0:BLK]
            A.g_key = x1t[:, BLK:2 * BLK]
            A.g_eP = ytmp[:, 0:BLK]
            A.g_eM = ytmp[:, BLK:2 * BLK]
            A.g_lf = tmpf[:]
            A.g_cum = numt[:]
            g1bc = sb("g1bc", [128, DM], F32)
            A.kt_ = [sb("kt_%d" % h, [128, BLK], BF16) for h in range(4)]
            A.qm0 = [sb("qm0%d" % h, [128, BLK], BF16) for h in range(4)]
            A.qm1 = [sb("qm1%d" % h, [128, BLK], BF16) for h in range(4)]
            A.dtot = [sb("dtot%d" % h, [128, 8], F32) for h in range(4)]
            A.sTm = [sb("sTm%d" % h, [128, 128], BF16) for h in range(4)]
            A.ktT0 = [sb("ktT0%d" % h, [128, 128], BF16) for h in range(4)]
            A.ktT1 = [sb("ktT1%d" % h, [128, 128], BF16) for h in range(4)]
            A.Shat = [[sb("Shat%d%d" % (h, c), [128, 128], BF16) for c in range(2)] for h in range(4)]
            Sf = [sb("Sf%d" % h, [128, 128], F32) for h in range(4)]
            Sb = [sb("Sb%d" % h, [128, 128], F32) for h in range(4)]
            of = [sb("of%d" % i, [128, 512], BF16) for i in range(16)]
            kdup = [sb("kdup%d" % i, [128, NKEY], BF16) for i in range(2)]
            vext = [sb("vext%d" % i, [128, 2, 192], BF16) for i in range(NKEY // 128)]
            A.ropeC = sb("ropeC", [128, BLK], F32)
            A.ropeS = sb("ropeS", [128, BLK], F32)
            A.qk_sq = sb("qk_sq", [128, BLK], BF16)
            A.qk_ms = osum[:]
            A.qk_rr = u1[:]
            A.qk_kn = x1t[:, 0:BLK]
            A.qk_knb = sb("qk_knb", [128, BLK], BF16)
            sgl = [sb("sgl%d" % i, [128, 512], BF16) for i in range(4)]
            ub = sb("ub", [128, 512], BF16)
            ss4 = sb("ss4", [128, 4], F32)
            ss4b = sb("ss4b", [128, 4], F32)
            rs4 = sb("rs4", [128, 4], F32)
            uT = [sb("uT%d" % k, [128, BLK], BF16) for k in range(4)]
            pT = [sb("pT%d" % i, [128, BLK], BF16) for i in range(3)]
            Dt = sb("Dt", [128, BLK], F32)
            OT = [sb("OT%d" % k, [128, BLK], BF16) for k in range(4)]
            sgA = [sb("sgA%d" % i, [128, BLK], BF16) for i in range(2)]
            mT = [sb("mT%d" % k, [128, BLK], BF16) for k in range(8)]
            h2f = [sb("h2f%d" % k, [128, 128], F32) for k in range(8)]
            lgt = sb("lgt", [128, NEXP], F32)
            lmx = sb("lmx", [128, 1], F32)
            lsm = sb("lsm", [128, 1], F32)
            aff = sb("aff", [128, 16, NEXP], F32)
            A.make_gbc(s, 2 * DM, g1bc, ph)
            if A.dbg == "tm_ca":
                A.dump("g1bc", g1bc[:], 1024)
                P.flush()
                return

            xi = [0]

            def load_x(src_ap):
                t = xts[xi[0] % 2]
                xi[0] += 1
                P.dma("sp", t[:], src_ap, key="x_" + t.name)
                return t

            for kt in range(NKEY // 128):
                A.memset(vext[kt][:, :, 64:128], 1.0)
            for h in range(4):
                A.memset(Sf[h][:], 0.0)
                A.memset(Sb[h][:], 0.0)
            A.memset(Dt[:], 1.0)
            for h in range(4):
                A.memset(A.qm0[h][:], 0.0)
                A.memset(A.qm1[h][:], 0.0)
                A.memset(A.ktT0[h][:], 0.0)
                A.memset(A.ktT1[h][:], 0.0)

            if A.dbg == "tm_c0":
                A.dump("g1bc", g1bc[:], 1024)
                P.flush()
                return
            for tt in range(2):
                xt = load_x(D["ctx"][s, tt * 128:(tt + 1) * 128, :])
                A.norm_to_hT(xt, 2, hT, tt * 128, A.sc1T, 0)
            if A.dbg == "tm_c1":
                A.dump("xt", xts[1][:, 0:256], 256)
                A.dump("hT0", hT[0][:, 0:256], 256)
                P.flush()
                return
            n = CTX
            w_ff = A.win_group(C_FF)
            w_fb = A.win_group(C_FB)
            w_i = A.win_group(C_I)
            for tt in range(2):
                A.proj_tm(A.pX[:], hT, tt * 128, w_i, 0, 512)
                A.cp(vi[tt][:], A.pX[:], eng="act")
            for h in range(4):
                A.proj_fm(A.pj0, w_ff, h * 128, hT, n)
                A.gates(0, h, A.pj0, None, n)
            if A.dbg == "tm_c2":
                A.dump("kt0", A.kt_[0][:, 0:256], 256)
                A.dump("vi0", vi[0][:], 512)
                P.flush()
                return
            for tt in range(2):
                A.hgrn_tile(0, tt, vi[tt], Sf, False)
            if A.dbg == "tm_c3":
                A.dump("Sf0", Sf[0][:], 128)
                P.flush()
                return
            for h in range(4):
                A.proj_fm(A.pj0, w_fb, h * 128, hT, n)
                A.gates(1, h, A.pj0, None, n)
            for tt in (1, 0):
                A.hgrn_tile(1, tt, vi[tt], Sb, False)
            for kvh in range(2):
                A.proj_fm(A.pj0, A.wk[kvh], 0, hT, n)
                A.qknorm(A.pj0, n, A.kng, kdup[kvh][:, 0:n], None)
            for tt in range(2):
                A.proj_tm(A.pX[:, 0:128], hT, tt * 128, A.wv, 0, 128)
                for kvh in range(2):
                    A.cp(vext[tt][:, kvh, 0:64], A.pX[:, kvh * 64:(kvh + 1) * 64], eng="act")
                    A.cp(vext[tt][:, kvh, 128:192], A.pX[:, kvh * 64:(kvh + 1) * 64], eng="dve")
            if A.dbg == "tm_ctx":
                A.dump("Sf0", Sf[0][:], 128)
                A.dump("Sb3", Sb[3][:], 128)
                A.dump("kdup1c", kdup[1][:, 0:256], 256)
                A.dump("vext1", vext[1][:].rearrange("p a b -> p (a b)"), 384)
                A.dump("hT0", hT[0][:, 0:256], 256)
                P.flush()
                return

            for b in range(NBLK):
                tok0 = b * BLK
                for tt in range(4):
                    xt = load_x(D["x"][s, tok0 + tt * 128:tok0 + (tt + 1) * 128, :])
                    A.norm_to_hT(xt, s, hT, tt * 128, A.sc1T, 0)
                P.dma("sp", A.ropeC[:], D["c_ropeC"][:, tok0:tok0 + BLK], key="ropeC")
                P.dma("sp", A.ropeS[:], D["c_ropeS"][:, tok0:tok0 + BLK], key="ropeS")
                w_ff = A.win_group(C_FF)
                w_q = A.win_group(C_Q)
                w_i = A.win_group(C_I)
                for h in range(4):
                    A.proj_fm(A.pj0, w_ff, h * 128, hT, BLK)
                    A.proj_fm(A.pj1, w_q, h * 128, hT, BLK)
                    A.gates(0, h, A.pj0, A.pj1, BLK)
                for tt in range(4):
                    A.proj_tm(A.pX[:], hT, tt * 128, w_i, 0, 512)
                    A.cp(vi[tt][:], A.pX[:], eng="act")
                for tt in range(4):
                    A.hgrn_tile(0, tt, vi[tt], Sf, True)
                    A.cp(of[b * 4 + tt][:], A.po[:], eng="act")
                for kvh in range(2):
                    A.proj_fm(A.pj0, A.wk[kvh], 0, hT, BLK)
                    A.qknorm(A.pj0, BLK, A.kng, kdup[kvh][:, CTX + tok0:CTX + tok0 + BLK], 0)
                for tt in range(4):
                    kt = 2 + b * 4 + tt
                    A.proj_tm(A.pX[:, 0:128], hT, tt * 128, A.wv, 0, 128)
                    for kvh in range(2):
                        A.cp(vext[kt][:, kvh, 0:64], A.pX[:, kvh * 64:(kvh + 1) * 64], eng="act")
                        A.cp(vext[kt][:, kvh, 128:192], A.pX[:, kvh * 64:(kvh + 1) * 64], eng="dve")
                if A.dbg == "tm_f0" and b == 0:
                    A.dump("of0", of[0][:], 512)
                    A.dump("of3", of[3][:], 512)
                    A.dump("kdup0", kdup[0][:, CTX:CTX + 512], 512)
                    A.dump("kt0", A.kt_[0][:], 512)
                    A.dump("vi0", vi[0][:], 512)
                    P.flush()
                    return
            if A.dbg == "tm_f":
                A.dump("of15", of[15][:], 512)
                A.dump("Sf1", Sf[1][:], 128)
                P.flush()
                return

            w_in3 = D["w_in"].rearrange("(kc p) j -> p kc j", p=128)
            for b in range(NBLK - 1, -1, -1):
                tok0 = b * BLK
                for tt in range(4):
                    xt = load_x(D["x"][s, tok0 + tt * 128:tok0 + (tt + 1) * 128, :])
                    A.norm_to_hT(xt, s, hT, tt * 128, A.sc1T, 0)
                P.dma("sp", A.ropeC[:], D["c_ropeC"][:, tok0:tok0 + BLK], key="ropeC")
                P.dma("sp", A.ropeS[:], D["c_ropeS"][:, tok0:tok0 + BLK], key="ropeS")
                w_fb = A.win_group(C_FB)
                w_q = A.win_group(C_Q)
                w_i = A.win_group(C_I)
                for h in range(4):
                    A.proj_fm(A.pj0, w_fb, h * 128, hT, BLK)
                    A.proj_fm(A.pj1, w_q, h * 128, hT, BLK)
                    A.gates(1, h, A.pj0, A.pj1, BLK)
                w_g = A.win_group(C_G)
                for tt in range(4):
                    A.proj_tm(A.pX[:], hT, tt * 128, w_i, 0, 512)
                    A.cp(vi[tt][:], A.pX[:], eng="act")
                    A.proj_tm(A.pj0[:], hT, tt * 128, w_g, 0, 512)
                    A.act(sgl[tt][:], A.pj0[:], AF.Silu)
                for tt in (3, 2, 1, 0):
                    A.hgrn_tile(1, tt, vi[tt], Sb, True)
                    gt = b * 4 + tt
                    A.tt(osum[:], A.po[:], of[gt][:], ALU.add)
                    for h in range(4):
                        A.act(A.junk[:, h * 128:(h + 1) * 128], osum[:, h * 128:(h + 1) * 128], AF.Square,
                              accum_out=ss4[:, h:h + 1])
                    A.rstd(rs4[:], ss4[:], 1.0 / 128, ss4b[:])
                    for h in range(4):
                        hs = slice(h * 128, (h + 1) * 128)
                        A.stt(u1[:, hs], osum[:, hs], rs4[:, h:h + 1], A.hgn_bc[:, hs], ALU.mult, ALU.mult)
                    A.tt(ub[:], u1[:], sgl[tt][:], ALU.mult)
                    for kc in range(4):
                        A.tr(A.ptr[:, kc * 128:(kc + 1) * 128], ub[:, kc * 128:(kc + 1) * 128], A.ident_b[:])
                    for kc in range(4):
                        A.cp(uT[kc][:, tt * 128:(tt + 1) * 128], A.ptr[:, kc * 128:(kc + 1) * 128],
                             eng=("act" if kc % 2 == 0 else "dve"))
                if A.dbg == "tm_b3" and b == NBLK - 1:
                    A.dump("uT0", uT[0][:], 512)
                    A.dump("uT3", uT[3][:], 512)
                    P.flush()
                    return
                w_aq = A.win_group(C_AQ)
                for qc in range(4):
                    A.memset(mT[qc][64:128, :], 0.0)
                    A.memset(mT[4 + qc][0:64, :], 0.0)
                    A.proj_fm(A.pj0, w_aq, qc * 128, hT, BLK)
                    A.qknorm(A.pj0, BLK, A.qng, (mT[qc], mT[4 + qc]), 0)
                NKT = NKEY // 128
                seq = [(qc, hh, kt) for qc in range(4) for hh in range(2) for kt in range(NKT)]
                pSs = (A.pj0, A.pj1)
                accs = (A.pX, A.bS)
                fxs = (A.po, A.bKV)

                def emit_S(i):
                    qc_, hh_, kt_ = seq[i]
                    A.mm(pSs[i % 2][:], kdup[qc_ // 2][:, kt_ * 128:(kt_ + 1) * 128], mT[hh_ * 4 + qc_][:])

                emit_S(0)
                for i, (qc, hh, kt) in enumerate(seq):
                    kvh = qc // 2
                    g = i // NKT
                    acc = accs[g % 2]
                    if i + 1 < len(seq):
                        emit_S(i + 1)
                    pt = pT[i % 3]
                    A.act(pt[:], pSs[i % 2][:], AF.Exp, scale=0.125)
                    A.mm(acc[:], vext[kt][:, kvh, hh * 64:hh * 64 + 128], pt[:],
                         start=(kt == 0), stop=(kt == NKT - 1))
                    if kt == NKT - 1:
                        rows = slice(hh * 64, (hh + 1) * 64)
                        den = slice((1 - hh) * 64, (2 - hh) * 64)
                        fx = fxs[g % 2]
                        A.recip(Dt[den, :], acc[den, :])
                        A.mm(fx[:], A.rot64[:], Dt[:])
                        A.cp(numt[rows, :], acc[rows, :], eng="act")
                        A.tt(OT[qc][rows, :], numt[rows, :], fx[rows, :], ALU.mult)
                if A.dbg == "tm_att" and b == NBLK - 1:
                    A.dump("qn0a", mT[0][:], 512)
                    A.dump("qn0b", mT[4][:], 512)
                    A.dump("OT0", OT[0][:], 512)
                    A.dump("OT3", OT[3][:], 512)
                    P.flush()
                    return
                w_a = A.ring_load(D["w_branch_a"].rearrange("(kc p) m -> p kc m", p=128))
                w_a4 = w_a[:].rearrange("p a b -> p (a b)").rearrange("p (a b) -> p a b", b=DM)
                for half in range(2):
                    w_ga = A.win_group(C_GA + half * 512)
                    for j in range(4):
                        dmc = half * 4 + j
                        A.proj_fm(A.pj0, w_ga, j * 128, hT, BLK)
                        sg_ = sgA[dmc % 2]
                        A.act(sg_[:], A.pj0[:], AF.Sigmoid)
                        for kc in range(4):
                            A.mm(A.pj1[:], w_a4[:, kc, dmc * 128:(dmc + 1) * 128], uT[kc][:], start=(kc == 0),
                                 stop=(kc == 3))
                        A.tt(mT[dmc][:], A.pj1[:], sg_[:], ALU.mult)
                w_b = A.ring_load(D["w_branch_b"].rearrange("(kc p) m -> p kc m", p=128))
                w_b4 = w_b[:].rearrange("p a b -> p (a b)").rearrange("p (a b) -> p a b", b=DM)
                for half in range(2):
                    w_gb = A.win_group(C_GB + half * 512)
                    for j in range(4):
                        dmc = half * 4 + j
                        A.proj_fm(A.pj0, w_gb, j * 128, hT, BLK)
                        sg_ = sgA[dmc % 2]
                        A.act(sg_[:], A.pj0[:], AF.Sigmoid)
                        for kc in range(4):
                            A.mm(A.pj1[:], w_b4[:, kc, dmc * 128:(dmc + 1) * 128], OT[kc][:], start=(kc == 0),
                                 stop=(kc == 3))
                        A.tt(tmpf[:], A.pj1[:], sg_[:], ALU.mult)
                        A.tt(mT[dmc][:], tmpf[:], mT[dmc][:], ALU.add)
                w_o = [A.ring_load(D["w_out"].rearrange("(kc p) m -> p kc m", p=128)[:, :, hf * 512:(hf + 1) * 512])
                       for hf in range(2)]
                for tt in range(4):
                    gt = b * 4 + tt
                    xt = load_x(D["x"][s, tok0 + tt * 128:tok0 + (tt + 1) * 128, :])
                    for hf in range(2):
                        pyo = A.pj0 if hf == 0 else A.pj1
                        for kc in range(8):
                            A.mm(pyo[:], mT[kc][:, tt * 128:(tt + 1) * 128], w_o[hf][:, kc, :], start=(kc == 0),
                                 stop=(kc == 7))
                        A.tt(ytmp[:, hf * 512:(hf + 1) * 512], pyo[:], g1bc[:, hf * 512:(hf + 1) * 512], ALU.mult)
                    A.tt(x1t[:], ytmp[:], xt[:], ALU.add)
                    P.dma("sp", D["out"][s, tok0 + tt * 128:tok0 + (tt + 1) * 128, :], x1t[:], key="x1st")
                    A.act(A.junk[:, 0:DM], x1t[:], AF.Square, accum_out=A.n_ss[:])
                    A.rstd(A.n_rs[:], A.n_ss[:], 1.0 / DM, A.n_ss2[:])
                    A.ts(ytmp[:], x1t[:], A.n_rs[:, 0:1], None, ALU.mult)
                    A.cp(A.xn[:], ytmp[:])
                    P.dma("sp", h2s[s, gt], A.xn[:], key="xn2st")
                    for kc in range(8):
                        pq_ = A.pj0 if kc < 4 else A.pj1
                        A.tr(pq_[:, (kc % 4) * 128:(kc % 4 + 1) * 128], ytmp[:, kc * 128:(kc + 1) * 128], A.ident_f[:])
                    for kc in range(8):
                        pq_ = A.pj0 if kc < 4 else A.pj1
                        src = pq_[:, (kc % 4) * 128:(kc % 4 + 1) * 128]
                        A.act(h2f[kc][:], src, AF.Identity, scale=A.sc2T[:, kc, s:s + 1], bias=A.modT[:, 24 + kc, s:s + 1])
                    for kc in range(8):
                        A.mm(A.pY[:, 0:NEXP], h2f[kc][:], A.wr[:, kc, :], start=(kc == 0), stop=(kc == 7))
                    A.P.op("dve", lambda e: e.reduce_max(out=lmx[:], in_=A.pY[:, 0:NEXP], axis=AX.X),
                           outs=[lmx[:]], ins=[A.pY[:, 0:NEXP]])
                    A.ts(lmx[:], lmx[:], -1.0, None, ALU.mult)
                    A.act(lgt[:], A.pY[:, 0:NEXP], AF.Exp, bias=lmx[:, 0:1], accum_out=lsm[:])
                    A.recip(lsm[:], lsm[:])
                    A.ts(aff[:, gt, :], lgt[:], lsm[:, 0:1], None, ALU.mult)
                if A.dbg == "tm_b3full" and b == NBLK - 1:
                    A.dump("x1", x1t[:], 1024)
                    A.dump("mT0", mT[0][:], 512)
                    A.dump("aff", aff[:, 15, :], 16)
                    P.flush()
                    return

            for gt in range(16):
                A.tr(A.pY[0:NEXP, 128:256], aff[:, gt, :], A.ident_f[:])
                A.cp(affT[:, gt * 128:(gt + 1) * 128], A.pY[0:NEXP, 128:256])
            lo = sb("lo", [NEXP, 1], F32)
            mid = sb("mid", [NEXP, 1], F32)
            cnt = sb("cnt", [NEXP, 1], F32)
            ge = sb("ge", [NEXP, 1], F32)
            cj = kdup[0][0:NEXP, 0:L]
            A.memset(lo[:], 0.0)
            for k in range(1, 29):
                wk_ = 2.0 ** (-k)
                A.ts(mid[:], lo[:], wk_, None, ALU.add)
                A.ts(cj, affT, mid[:, 0:1], 0.0, ALU.is_ge, ALU.add, accum_out=cnt[:])
                A.ts(ge[:], cnt[:], float(CAP), wk_, ALU.is_ge, ALU.mult)
                A.tt(lo[:], lo[:], ge[:], ALU.add)
            dg = sb("dg", [NEXP, NEXP], F32)
            ones16 = sb("ones16", [NEXP, 128], F32)
            thr = sb("thr", [128, 1, NEXP], F32)
            A.memset(ones16[:], 1.0)
            A.ts(dg[:], A.ident_f[0:NEXP, 0:NEXP], lo[:, 0:1], None, ALU.mult)
            A.mm(A.pY[:, 0:NEXP], ones16[:], dg[:])
            A.cp(thr[:, 0, :], A.pY[:, 0:NEXP])
            msk = sb("msk", [128, 16, NEXP], F32)
            A.tt(msk[:], aff[:], thr[:].to_broadcast([128, 16, NEXP]), ALU.is_ge)
            A.tt(A.wgt[s][:], msk[:], aff[:], ALU.mult)
            if A.dbg == "tm_all":
                A.dump("wgt", A.wgt[s][:].rearrange("p a b -> p (a b)"), 256)
                A.dump("aff", aff[:].rearrange("p a b -> p (a b)"), 256)
            P.flush()

    def moe(self, s, xn2s):
        A = self
        P = A.P
        D = A.D
        NT = L // 128
        with ExitStack() as ph:
            A.ph = ph
            sb = lambda n, shp, dt: P.sb(n, shp, dt, ph)
            ering = [sb("ering%d" % i, [128, 8, DM], BF16) for i in range(3)]
            acc = [sb("acc%d" % i, [128, DM], F32) for i in range(NT)]
            xq = [sb("xq%d" % i, [128, 4, DM], BF16) for i in range(4)]
            Sel = [sb("Sel%d" % i, [128, CAP], BF16) for i in range(NT)]
            SelT = [sb("SelT%d" % i, [128, 1024], BF16) for i in range(2)]
            hid = [sb("hid%d" % i, [128, CAP], BF16) for i in range(8)]
            xeT = [sb("xeT%d" % i, [128, CAP], BF16) for i in range(8)]
            yw = [sb("yw%d" % i, [128, DM], BF16) for i in range(2)]
            g2bc = sb("g2bc", [128, DM], F32)
            fing_bc = sb("fing_bc", [128, DM], F32)
            mf = sb("mf", [128, NT, NEXP], F32)
            mb = sb("mb", [128, NT, NEXP], BF16)
            pm = sb("pm", [128, NT, NEXP], F32)
            pmT = sb("pmT", [NEXP, L], BF16)
            rowt = sb("rowt", [NEXP, 512], BF16)
            ones16 = sb("ones16b", [NEXP, 128], BF16)
            w2 = sb("w2", [128, NT, NEXP, 2], BF16)
            wtmp = pm
            x1r = SelT[0][:].bitcast(F32)
            g4 = sb("g4", [128, 4], F32)
            gc = sb("gc", [128, 2], F32)
            ptmp = sb("ptmp", [128, NEXP], F32)
            P.dma("sp", fing_bc[:], D["final_norm_g"].partition_broadcast(128), key="fing")
            A.make_gbc(s, 5 * DM, g2bc, ph, slot_ap=ering[0][:, :, 0:512], asb=ering[1][:, :, 0:128])
            for q in range(4):
                P.dma("sp", xq[q][:], xn2s[s, q * 4:(q + 1) * 4].rearrange("t p d -> p t d"), key="xq%d" % q)
            A.memset(ones16[:], 1.0)
            wg_ = A.wgt[s]
            A.ts(mf[:], wg_[:], 0.0, None, ALU.is_gt)
            A.cp(mb[:], mf[:])
            A.cp(w2[:, :, :, 0], wg_[:])
            A.tt(wtmp[:], wg_[:], w2[:, :, :, 0], ALU.subtract)
            A.cp(w2[:, :, :, 1], wtmp[:])
            for tt in range(NT):
                A.mm(A.pY[:, 0:NEXP], A.tri_b[:], mb[:, tt, :], start=True, stop=(tt == 0))
                for t2 in range(tt):
                    A.mm(A.pY[:, 0:NEXP], A.ones_b[:], mb[:, t2, :], start=False, stop=(t2 == tt - 1))
                A.tt(ptmp[:], A.pY[:, 0:NEXP], mf[:, tt, :], ALU.mult)
                A.ts(pm[:, tt, :], ptmp[:], -1.0, None, ALU.add)
                A.tr(A.pY[0:NEXP, 128:256], pm[:, tt, :], A.ident_f[:])
                A.cp(pmT[:, tt * 128:(tt + 1) * 128], A.pY[0:NEXP, 128:256])
            eri = [0]

            def eload(src_ap):
                i = eri[0] % len(ering)
                eri[0] += 1
                P.dma("pool", ering[i][:], src_ap.rearrange("(kc p) f -> p kc f", p=128), key="ering%d" % i)
                return ering[i]

            pgs = (A.pX, A.po)
            gi = 0
            for e in range(NEXP):
                wg = eload(D["w_exp_gate"][e])
                wu = eload(D["w_exp_up"][e])
                for tt in range(NT):
                    A.ts(Sel[tt][:], A.iota[:], pm[:, tt, e:e + 1], None, ALU.is_equal)
                for kc in range(8):
                    pg = pgs[gi % 2]
                    gi += 1
                    for tt in range(NT):
                        A.mm(pg[:, 0:CAP], xq[tt // 4][:, tt % 4, kc * 128:(kc + 1) * 128], Sel[tt][:],
                             start=(tt == 0), stop=(tt == NT - 1))
                    A.act(xeT[kc][:], pg[:, 0:CAP], AF.Identity, scale=A.sc2T[:, kc, s:s + 1],
                          bias=A.modT[:, 24 + kc, s:s + 1])
                for ct in range(2):
                    for tt in range(NT):
                        A.mm(A.pY[:, ct * 2:(ct + 1) * 2], Sel[tt][:, ct * 128:(ct + 1) * 128], w2[:, tt, e, :],
                             start=(tt == 0), stop=(tt == NT - 1))
                A.cp(g4[:], A.pY[:, 0:4])
                for ct in range(2):
                    A.tt(gc[:, ct:ct + 1], g4[:, 2 * ct:2 * ct + 1], g4[:, 2 * ct + 1:2 * ct + 2], ALU.add)
                for fc in range(8):
                    for kc in range(8):
                        A.mm(A.pj0[:, 0:CAP], wg[:, kc, fc * 128:(fc + 1) * 128], xeT[kc][:], start=(kc == 0), stop=(kc == 7))
                    for kc in range(8):
                        A.mm(A.pj1[:, 0:CAP], wu[:, kc, fc * 128:(fc + 1) * 128], xeT[kc][:], start=(kc == 0), stop=(kc == 7))
                    A.act(hid[fc][:], A.pj0[:, 0:CAP], AF.Silu)
                    A.tt(hid[fc][:], hid[fc][:], A.pj1[:, 0:CAP], ALU.mult)
                wd = eload(D["w_exp_down"][e])
                for ct in range(2):
                    for hf in range(2):
                        py = pgs[gi % 2]
                        gi += 1
                        for fc in range(8):
                            A.mm(py[:], hid[fc][:, ct * 128:(ct + 1) * 128], wd[:, fc, hf * 512:(hf + 1) * 512],
                                 start=(fc == 0), stop=(fc == 7))
                        A.ts(yw[ct][:, hf * 512:(hf + 1) * 512], py[:], gc[:, ct:ct + 1], None, ALU.mult)
                for th in range(2):
                    for q in range(2):
                        blk = th * 2 + q
                        A.ts(rowt[:], pmT[:, blk * 512:(blk + 1) * 512], A.ident_f[0:NEXP, e:e + 1], None, ALU.mult)
                        pb = A.bS if q == 0 else A.bKV
                        A.mm(pb[:], ones16[:], rowt[:])
                        for ct in range(2):
                            A.ts(SelT[ct][:, q * 512:(q + 1) * 512], pb[:], A.iotap[:, ct:ct + 1], None, ALU.is_equal)
                    for tl in range(8):
                        gt = th * 8 + tl
                        for hf in range(2):
                            py = pgs[gi % 2]
                            gi += 1
                            for ct in range(2):
                                A.mm(py[:], SelT[ct][:, tl * 128:(tl + 1) * 128], yw[ct][:, hf * 512:(hf + 1) * 512],
                                     start=(ct == 0), stop=(ct == 1))
                            dst = acc[gt][:, hf * 512:(hf + 1) * 512]
                            if e == 0:
                                A.cp(dst, py[:], eng="act")
                            else:
                                A.tt(dst, py[:], dst, ALU.add)
            for gt in range(NT):
                tok = gt * 128
                fin = acc[gt]
                A.tt(fin[:], fin[:], g2bc[:], ALU.mult)
                for hf in range(2):
                    P.dma("sp", x1r, D["out"][s, tok:tok + 128, hf * 512:(hf + 1) * 512], key="x1ld")
                    A.tt(fin[:, hf * 512:(hf + 1) * 512], fin[:, hf * 512:(hf + 1) * 512], x1r, ALU.add)
                A.act(A.junk[:, 0:DM], fin[:], AF.Square, accum_out=A.n_ss[:])
                A.rstd(A.n_rs[:], A.n_ss[:], 1.0 / DM, A.n_ss2[:])
                A.stt(fin[:], fin[:], A.n_rs[:, 0:1], fing_bc[:], ALU.mult, ALU.mult)
                P.dma("sp", D["out"][s, tok:tok + 128, :], fin[:], key="ost%d" % gt)
            P.flush()


def _in_maps(inputs):
    consts = host_consts()
    maps = []
    f = lambda a: np.ascontiguousarray(np.asarray(a, dtype=np.float32))
    shared = {
        "c_ctx": f(inputs["c_ctx"]), "w_mod": f(inputs["w_mod"][0]), "b_mod": f(inputs["b_mod"][0]),
        "norm1_g": f(inputs["norm1_g"][0]), "norm2_g": f(inputs["norm2_g"][0]), "w_in": f(inputs["w_in"][0]),
        "hg_lb_logits": f(np.asarray(inputs["hg_lb_logits"])[:, 0:2, :]), "hg_norm_g": f(inputs["hg_norm_g"][0]),
        "q_norm_g": f(inputs["q_norm_g"][0]), "k_norm_g": f(inputs["k_norm_g"][0]),
        "w_branch_a": f(inputs["w_branch_a"][0]), "w_branch_b": f(inputs["w_branch_b"][0]),
        "w_out": f(inputs["w_out"][0]), "w_router": f(inputs["w_router"][0]),
        "w_exp_gate": f(inputs["w_exp_gate"][0]), "w_exp_up": f(inputs["w_exp_up"][0]),
        "w_exp_down": f(inputs["w_exp_down"][0]), "final_norm_g": f(inputs["final_norm_g"]),
    }
    shared.update(consts)
    x = np.asarray(inputs["x"], dtype=np.float32)
    c = np.asarray(inputs["c"], dtype=np.float32)
    ctx = np.asarray(inputs["ctx"], dtype=np.float32)
    for i in range(NCORES):
        m = dict(shared)
        m["x"] = np.ascontiguousarray(x[i * SPC:(i + 1) * SPC])
        m["c"] = np.ascontiguousarray(c[i * SPC:(i + 1) * SPC])
        m["ctx"] = np.ascontiguousarray(ctx[i * SPC:(i + 1) * SPC])
        maps.append(m)
    return maps


def kernel(**inputs):
    maps = _in_maps(inputs)
    nc = Builder().build()
    res = run_bass_kernel_spmd(nc, maps, core_ids=list(range(NCORES)))
    out = np.concatenate([np.asarray(r["out"], dtype=np.float32) for r in res.results], axis=0)
    return out
```

```python
import numpy as np
from contextlib import ExitStack
import concourse.bass as bass
import concourse.mybir as mybir
from concourse.bass_utils import run_bass_kernel_spmd

F32 = mybir.dt.float32
BF16 = mybir.dt.bfloat16
AF = mybir.ActivationFunctionType
ALU = mybir.AluOpType
AX = mybir.AxisListType

NCORES = 8
SPC = 2
L = 2048
DM = 1024
CTX = 256
NKEY = CTX + L
EPS = 1e-6
NEXP = 16
CAP = 256
BLK = 512
NBLK = L // BLK
C_Q, C_FF, C_FB, C_I, C_G, C_AQ, C_AK, C_AV, C_GA, C_GB = 0, 512, 1024, 1536, 2048, 2560, 3072, 3200, 3328, 4352

ENG_NAMES = ("pe", "act", "dve", "pool", "sp")
EPOCH = 8000
SAME_ENGINE_SYNC = {"pe": False, "act": True, "dve": True, "pool": True, "sp": True}


class Tile:
    __slots__ = ("name", "h", "last_w", "readers", "psum")

    def __init__(self, name, h, psum=False):
        self.name = name
        self.h = h
        self.last_w = None
        self.readers = []
        self.psum = psum

    def __getitem__(self, idx):
        return self.h[idx]


class Prog:
    def __init__(self, nc, stack):
        self.nc = nc
        self.stack = stack
        self.reg = {}
        self.ins = []
        self.base = 0
        self.dma_count = {}
        self.sems = {}
        self.eng_count = {e: 0 for e in ENG_NAMES}
        self.seen = {e: {} for e in ENG_NAMES}
        self.engs = {"pe": nc.tensor, "act": nc.scalar, "dve": nc.vector, "pool": nc.gpsimd, "sp": nc.sync}
        self.n_wait = 0
        self.uid = 0

    def sb(self, name, shape, dtype, stack=None):
        self.uid += 1
        nm = "%s_%d" % (name, self.uid)
        h = (stack or self.stack).enter_context(self.nc.sbuf_tensor(nm, list(shape), dtype))
        t = Tile(nm, h)
        self.reg[nm] = t
        return t

    def ps(self, name, shape, dtype, stack=None):
        self.uid += 1
        nm = "%s_%d" % (name, self.uid)
        h = (stack or self.stack).enter_context(self.nc.psum_tensor(nm, list(shape), dtype))
        t = Tile(nm, h, psum=True)
        self.reg[nm] = t
        return t

    def pseudo(self, name):
        t = Tile(name, None)
        return t

    def _tiles(self, aps):
        out = []
        for a in aps:
            if a is None or isinstance(a, (int, float)):
                continue
            if isinstance(a, Tile):
                out.append(a)
                continue
            t = self.reg.get(a.tensor.name)
            if t is not None:
                out.append(t)
        return out

    def op(self, engine, fn, outs=(), ins=(), dma_key=None):
        reads = self._tiles(ins)
        writes = self._tiles(outs)
        idx = len(self.ins)
        deps = set()
        for t in reads:
            if t.last_w is not None:
                deps.add(t.last_w)
            if t.psum:
                for r in t.readers:
                    if self.ins[r]["engine"] != engine:
                        deps.add(r)
        for t in writes:
            if t.last_w is not None:
                deps.add(t.last_w)
            for r in t.readers:
                deps.add(r)
        deps.discard(idx)
        if dma_key in ("const", "constc"):
            deps = {d for d in deps if self.ins[d]["dma_key"] != dma_key}
        rec = dict(engine=engine, fn=fn, deps=sorted(d for d in deps if d >= self.base),
                   dma_key=dma_key, signal=False)
        if dma_key is not None:
            self.dma_count[dma_key] = self.dma_count.get(dma_key, 0) + 1
            rec["val"] = 16 * self.dma_count[dma_key]
            rec["sem"] = ("dma", dma_key)
        self.ins.append(rec)
        for t in writes:
            t.last_w = idx
            t.readers = []
        for t in reads:
            if t not in writes:
                t.readers.append(idx)
        return idx

    def dma(self, queue, out, in_, key, xr=(), xw=(), **kw):
        return self.op(queue, lambda e: e.dma_start(out=out, in_=in_, **kw),
                       outs=[out] + list(xw), ins=[in_] + list(xr), dma_key=key)

    def _sem(self, k):
        if k not in self.sems:
            nm = "s_" + "_".join(str(x) for x in k)
            self.sems[k] = self.stack.enter_context(self.nc.semaphore(nm))
        return self.sems[k]

    def _wait(self, ename, d):
        rd = self.ins[d]
        k, v = rd["sem"], rd["val"]
        if self.seen[ename].get(k, 0) >= v:
            return
        self.engs[ename].wait_ge(self._sem(k), v)
        self.n_wait += 1
        self.seen[ename][k] = v

    def flush(self, barrier_engine="sp"):
        ins = self.ins
        lo, hi = self.base, len(ins)
        for i in range(lo, hi):
            r = ins[i]
            for d in r["deps"]:
                rd = ins[d]
                if rd["dma_key"] is not None or rd["engine"] != r["engine"] or SAME_ENGINE_SYNC[r["engine"]]:
                    rd["signal"] = True
        last_of = {}
        lastdma = {}
        for i in range(lo, hi):
            r = ins[i]
            if r["dma_key"] is None:
                last_of[r["engine"]] = i
            else:
                r["signal"] = True
                lastdma[r["dma_key"]] = i
        for e, i in last_of.items():
            ins[i]["signal"] = True
        for i in range(lo, hi):
            r = ins[i]
            if r["dma_key"] in ("const", "constc"):
                r["val"] = 16 * self.dma_count[r["dma_key"]]
        for i in range(lo, hi):
            r = ins[i]
            if r["dma_key"] is None and r["signal"]:
                c = self.eng_count[r["engine"]]
                r["sem"] = ("eng", r["engine"], c // EPOCH)
                r["val"] = c % EPOCH + 1
                self.eng_count[r["engine"]] = c + 1
        for i in range(lo, hi):
            r = ins[i]
            ename = r["engine"]
            for d in r["deps"]:
                rd = ins[d]
                if rd["dma_key"] is None and rd["engine"] == ename and not SAME_ENGINE_SYNC[ename]:
                    continue
                self._wait(ename, d)
            bi = r["fn"](self.engs[ename])
            if r["signal"]:
                bi.then_inc(self._sem(r["sem"]), 16 if r["dma_key"] is not None else 1)
            r["fn"] = None
        for ename in ENG_NAMES:
            for e, i in last_of.items():
                if e != ename:
                    self._wait(ename, i)
            for k, i in lastdma.items():
                self._wait(ename, i)
        self.base = hi
        for t in self.reg.values():
            t.last_w = None
            t.readers = []


def host_consts():
    c = {}
    c["c_ident"] = np.eye(128, dtype=np.float32)
    s = np.arange(128)[:, None]
    t = np.arange(128)[None, :]
    same = (s // 64) == (t // 64)
    c["c_maskF"] = (same & (s <= t)).astype(np.float32)
    c["c_maskB"] = (same & (s >= t)).astype(np.float32)
    r = np.ones((128, 512), np.float32)
    r[:, ::64] = 0.0
    c["c_reset"] = r
    c["c_bones"] = ((s // 64) == (t // 64)).astype(np.float32)
    pm = np.zeros((128, 128), np.float32)
    for m in range(128):
        d = m % 32
        partner = m + 16 if d < 16 else m - 16
        pm[partner, m] = 1.0
    c["c_perm"] = pm
    rot = np.zeros((128, 128), np.float32)
    for m in range(128):
        rot[(m + 64) % 128, m] = 1.0
    c["c_rot64"] = rot
    tt = np.arange(L, dtype=np.float32)
    row = np.floor(tt / 64.0).astype(np.float32)
    col = (tt - row * 64.0).astype(np.float32)
    inv_freq = (np.float32(10000.0) ** (-np.arange(0, 32, 2, dtype=np.float32) / np.float32(32.0))).astype(np.float32)
    C = np.zeros((128, L), np.float32)
    S = np.zeros((128, L), np.float32)
    for p in range(128):
        d = p % 64
        a = d // 32
        j = d % 16
        second = (d % 32) >= 16
        pos = row if a == 0 else col
        ang = (pos * inv_freq[j]).astype(np.float32)
        C[p] = np.cos(ang)
        S[p] = np.sin(ang) * (1.0 if second else -1.0)
    c["c_ropeC"] = C
    c["c_ropeS"] = S
    c["c_tri"] = (s <= t).astype(np.float32)
    c["c_iota"] = np.tile(np.arange(256, dtype=np.float32)[None, :], (128, 1))
    c["c_iotap"] = np.stack([np.arange(128, dtype=np.float32), np.arange(128, dtype=np.float32) + 128], 1)
    return c


CONST_SHAPES = {"c_ident": (128, 128), "c_maskF": (128, 128), "c_maskB": (128, 128), "c_reset": (128, 512),
                "c_bones": (128, 128), "c_perm": (128, 128), "c_rot64": (128, 128),
                "c_ropeC": (128, L), "c_ropeS": (128, L), "c_tri": (128, 128), "c_iota": (128, 256),
                "c_iotap": (128, 2)}

IN_SHAPES = {
    "x": (SPC, L, DM), "c": (SPC, DM), "ctx": (SPC, CTX, DM), "c_ctx": (DM,),
    "w_mod": (DM, 6 * DM), "b_mod": (6 * DM,), "norm1_g": (DM,), "norm2_g": (DM,),
    "w_in": (DM, 5376), "hg_lb_logits": (2, 2, 512), "hg_norm_g": (512,), "q_norm_g": (64,), "k_norm_g": (64,),
    "w_branch_a": (512, DM), "w_branch_b": (512, DM), "w_out": (DM, DM), "w_router": (DM, NEXP),
    "w_exp_gate": (NEXP, DM, DM), "w_exp_up": (NEXP, DM, DM), "w_exp_down": (NEXP, DM, DM),
    "final_norm_g": (DM,),
}


class Builder:
    def __init__(self, dbg=None, nsamp=SPC):
        self.dbg = dbg
        self.nsamp = nsamp
        self.nc = bass.Bass("TRN2", target_bir_lowering=False)
        self.D = {}
        self.dbg_items = {}
        self.dbg_col = 0

    def act(self, out, in_, func, **kw):
        ins = [in_] + [v for v in kw.values() if not isinstance(v, (int, float))]
        outs = [out]
        if "accum_out" in kw:
            outs.append(kw["accum_out"])
        self.P.op("act", lambda e: e.activation(out=out, in_=in_, func=func, **kw), outs=outs, ins=ins)

    def tt(self, out, in0, in1, op, eng="dve"):
        self.P.op(eng, lambda e: e.tensor_tensor(out=out, in0=in0, in1=in1, op=op), outs=[out], ins=[in0, in1])

    def ts(self, out, in0, s1, s2, op0, op1=None, eng="dve", accum_out=None):
        kw = {}
        if op1 is not None:
            kw["op1"] = op1
        outs = [out]
        if accum_out is not None:
            kw["accum_out"] = accum_out
            outs.append(accum_out)
        self.P.op(eng, lambda e: e.tensor_scalar(out=out, in0=in0, scalar1=s1, scalar2=s2, op0=op0, **kw),
                  outs=outs, ins=[in0, s1, s2])

    def stt(self, out, in0, scalar, in1, op0, op1):
        self.P.op("dve", lambda e: e.scalar_tensor_tensor(out=out, in0=in0, scalar=scalar, in1=in1, op0=op0, op1=op1),
                  outs=[out], ins=[in0, scalar, in1])

    def cp(self, out, in_, eng="dve"):
        if eng == "act":
            self.P.op("act", lambda e: e.activation(out=out, in_=in_, func=AF.Copy), outs=[out], ins=[in_])
        else:
            self.P.op(eng, lambda e: e.tensor_copy(out=out, in_=in_), outs=[out], ins=[in_])

    def memset(self, ap, val, eng="dve"):
        self.P.op(eng, lambda e: e.memset(ap, val), outs=[ap], ins=[])

    def recip(self, out, in_):
        self.P.op("dve", lambda e: e.reciprocal(out=out, in_=in_), outs=[out], ins=[in_])

    def mm(self, out, lhsT, rhs, start=True, stop=True):
        self.P.op("pe", lambda e: e.matmul(out, lhsT=lhsT, rhs=rhs, start=start, stop=stop),
                  outs=[out], ins=[lhsT, rhs])

    def tr(self, out, in_, ident):
        self.P.op("pe", lambda e: e.transpose(out, in_, ident), outs=[out], ins=[in_, ident])

    def din(self, name, shape, dt=F32):
        self.D[name] = self.nc.dram_tensor(name, list(shape), dt, kind="ExternalInput").ap()
        return self.D[name]

    def dump(self, name, ap, n, parts=128):
        if self.dbg is None:
            return
        if getattr(self, "dbg_st_ph", None) is not self.ph:
            self.dbg_st = self.P.sb("dbgst", [128, 512], F32, self.ph)
            self.dbg_st_ph = self.ph
        st = self.dbg_st
        c0 = self.dbg_col
        self.dbg_items[name] = (c0, n)
        self.dbg_col += n
        for j in range(0, n, 512):
            w = min(512, n - j)
            self.memset(st[:, 0:w], 0.0)
            self.cp(st[0:parts, 0:w], ap[:, j:j + w])
            self.P.dma("sp", self.D["dbg"][:, c0 + j:c0 + j + w], st[:, 0:w], key="dbg")

    def rstd(self, out, in_, mul, tmp):
        self.ts(tmp, in_, mul, EPS, ALU.mult, ALU.add)
        self.act(tmp, tmp, AF.Sqrt)
        self.recip(out, tmp)

    def ring_load(self, src_ap):
        i = self.ring_i % len(self.ring)
        self.ring_i += 1
        slot = self.ring[i]
        shp = src_ap.shape
        if len(shp) == 3 and (shp[1], shp[2]) != (8, 512):
            dst = slot[:].rearrange("p a b -> p (a b)").rearrange("p (a b) -> p a b", b=shp[2])
        else:
            dst = slot[:]
        self.P.dma("pool", dst, src_ap, key="ring%d" % i)
        return slot

    def win_group(self, c0):
        return self.ring_load(self.D["w_in"].rearrange("(kc p) j -> p kc j", p=128)[:, :, c0:c0 + 512])

    def make_gbc(self, s, col0, dst, stack, slot_ap=None, asb=None):
        A = self
        P = A.P
        Asb = asb if asb is not None else P.sb("Asb", [128, 8, 128], BF16, stack)
        for kc in range(8):
            A.ts(Asb[:, kc, :], A.ones_b[:], A.scT[:, s, kc:kc + 1], None, ALU.mult)
        P.dma("sp", dst[:], A.D["b_mod"][col0:col0 + DM].partition_broadcast(128), key="gbc_" + dst.name)
        wmod3 = A.D["w_mod"].rearrange("(kc p) j -> p kc j", p=128)
        for half in range(2):
            if slot_ap is None:
                slot = A.ring_load(wmod3[:, :, col0 + half * 512:col0 + (half + 1) * 512])
            else:
                slot = slot_ap
                P.dma("pool", slot, wmod3[:, :, col0 + half * 512:col0 + (half + 1) * 512], key="gbcw")
            for kc in range(8):
                A.mm(A.pj1[:], Asb[:, kc, :], slot[:, kc, :], start=(kc == 0), stop=(kc == 7))
            A.tt(dst[:, half * 512:(half + 1) * 512], A.pj1[:], dst[:, half * 512:(half + 1) * 512], ALU.add)

    def proj_fm(self, pt, w3, c0, hT, n):
        for kc in range(8):
            self.mm(pt[:, 0:n], w3[:, kc, c0:c0 + 128], hT[kc][:, 0:n], start=(kc == 0), stop=(kc == 7))

    def proj_tm(self, pt_ap, hT, t0, w3, c0, ncols):
        for kc in range(8):
            self.mm(pt_ap, hT[kc][:, t0:t0 + 128], w3[:, kc, c0:c0 + ncols], start=(kc == 0), stop=(kc == 7))

    def norm_to_hT(self, xt, v, hT, col0, scT, shbase):
        A = self
        import os
        NS = int(os.environ.get("NORM_STOP", "9"))
        junk, ss, ss2, rs, xn, ptr = A.junk, A.n_ss, A.n_ss2, A.n_rs, A.xn, A.ptr
        if NS < 1:
            return
        A.act(junk[:, 0:DM], xt[:], AF.Square, accum_out=ss[:])
        if NS < 2:
            return
        A.rstd(rs[:], ss[:], 1.0 / DM, ss2[:])
        if NS < 3:
            return
        A.ts(xn[:], xt[:], rs[:, 0:1], None, ALU.mult)
        if NS < 4:
            return
        for kc in range(8):
            A.tr(ptr[:, kc * 128:(kc + 1) * 128], xn[:, kc * 128:(kc + 1) * 128], A.ident_b[:])
        if NS < 5:
            return
        for kc in range(8):
            o = hT[kc][:, col0:col0 + 128]
            i = ptr[:, kc * 128:(kc + 1) * 128]
            EM = os.environ.get("EVAC_MODE", "mix")
            if (kc % 2 == 0 and EM == "mix") or EM == "act":
                A.act(o, i, AF.Identity, scale=scT[:, kc, v:v + 1], bias=A.modT[:, shbase + kc, v:v + 1])
            else:
                A.ts(o, i, scT[:, kc, v:v + 1], A.modT[:, shbase + kc, v:v + 1], ALU.mult, ALU.add)

    def qknorm(self, pk, n, gain, dest, rope_c0=None):
        A = self
        sq, ms, rr, kn, knb = A.qk_sq, A.qk_ms, A.qk_rr, A.qk_kn, A.qk_knb
        A.act(sq[:, 0:n], pk[:, 0:n], AF.Square)
        A.mm(A.pj1[:, 0:n], A.bones[:], sq[:, 0:n])
        A.ts(ms[:, 0:n], A.pj1[:, 0:n], 1.0 / 64, EPS, ALU.mult, ALU.add)
        A.act(ms[:, 0:n], ms[:, 0:n], AF.Sqrt)
        A.recip(rr[:, 0:n], ms[:, 0:n])
        if rope_c0 is None:
            A.stt(dest, pk[:, 0:n], gain[:, 0:1], rr[:, 0:n], ALU.mult, ALU.mult)
            return
        A.stt(kn[:, 0:n], pk[:, 0:n], gain[:, 0:1], rr[:, 0:n], ALU.mult, ALU.mult)
        A.cp(knb[:, 0:n], kn[:, 0:n], eng="act")
        A.mm(A.pX[:, 0:n], A.perm[:], knb[:, 0:n])
        A.tt(rr[:, 0:n], A.pX[:, 0:n], A.ropeS[:, 0:n], ALU.mult)
        A.tt(kn[:, 0:n], kn[:, 0:n], A.ropeC[:, 0:n], ALU.mult)
        if isinstance(dest, tuple):
            A.tt(dest[0][0:64, 0:n], kn[0:64, 0:n], rr[0:64, 0:n], ALU.add)
            A.tt(dest[1][64:128, 0:n], kn[64:128, 0:n], rr[64:128, 0:n], ALU.add)
        else:
            A.tt(dest, kn[:, 0:n], rr[:, 0:n], ALU.add)

    def gates(self, d, h, pf, pq, n):
        A = self
        sg, lf, key, cum, eP, eM = A.g_sg, A.g_lf, A.g_key, A.g_cum, A.g_eP, A.g_eM
        nch = n // 64
        A.act(sg[:, 0:n], pf[:, 0:n], AF.Sigmoid)
        A.act(lf[:, 0:n], sg[:, 0:n], AF.Ln, scale=A.oml[:, d, h:h + 1], bias=A.lb[:, d, h:h + 1])
        A.ts(key[:, 0:n], sg[:, 0:n], A.noml[:, d, h:h + 1], A.oml[:, d, h:h + 1], ALU.mult, ALU.add)
        A.P.op("dve", lambda e: e.tensor_tensor_scan(out=cum[:, 0:n], data0=A.reset[:, 0:n], data1=lf[:, 0:n],
                                                     initial=0.0, op0=ALU.mult, op1=ALU.add),
               outs=[cum[:, 0:n]], ins=[A.reset[:, 0:n], lf[:, 0:n]])
        cv = cum[:, 0:n].rearrange("p (c k) -> p c k", k=64)
        A.act(A.dtot[h][:, 0:nch], cv[:, :, 63], AF.Exp)
        if d == 0:
            A.tt(lf[:, 0:n].rearrange("p (c k) -> p c k", k=64), cv, cv[:, :, 63:64].to_broadcast([128, nch, 64]),
                 ALU.subtract)
        else:
            A.tt(lf[:, 0:n], cum[:, 0:n], lf[:, 0:n], ALU.subtract)
        A.act(eP[:, 0:n], lf[:, 0:n], AF.Exp)
        A.act(eM[:, 0:n], lf[:, 0:n], AF.Exp, scale=-1.0)
        if d == 0:
            eq, ek = eP, eM
        else:
            eq, ek = eM, eP
        A.tt(A.kt_[h][:, 0:n], key[:, 0:n], ek[:, 0:n], ALU.mult)
        if pq is not None:
            v4 = lambda ap: ap[:, 0:n].rearrange("p (c two k) -> p c two k", two=2, k=64)
            A.tt(v4(A.qm0[h])[:, :, 0, :], v4(pq)[:, :, 0, :], v4(eq)[:, :, 0, :], ALU.mult)
            A.tt(v4(A.qm1[h])[:, :, 1, :], v4(pq)[:, :, 1, :], v4(eq)[:, :, 1, :], ALU.mult)

    def hgrn_tile(self, d, tt, vi_t, S, want_out):
        A = self
        t0 = tt * 128
        mask = A.maskF if d == 0 else A.maskB
        order = (0, 1) if d == 0 else (1, 0)
        for h in range(4):
            hs = slice(h * 128, (h + 1) * 128)
            ktile = A.kt_[h][:, t0:t0 + 128]
            q0 = A.qm0[h][:, t0:t0 + 128]
            q1 = A.qm1[h][:, t0:t0 + 128]
            if want_out:
                A.mm(A.psT[h][:], ktile, q0, start=True, stop=False)
                A.mm(A.psT[h][:], ktile, q1, start=False, stop=True)
                A.tt(A.sTm[h][:], A.psT[h][:], mask[:], ALU.mult)
            A.tr(A.ptk[h % 2][:], ktile, A.ident_b[:])
            A.cp(A.ktT0[h][0:64, :], A.ptk[h % 2][0:64, :], eng="act")
            A.cp(A.ktT1[h][64:128, :], A.ptk[h % 2][64:128, :], eng="dve")
            for cp_ in order:
                rows = slice(cp_ * 64, (cp_ + 1) * 64)
                ci = tt * 2 + cp_
                A.mm(A.pkv[h % 2][cp_][:], (A.ktT0 if cp_ == 0 else A.ktT1)[h][:], vi_t[:, hs])
                if want_out:
                    A.act(A.Shat[h][cp_][:], S[h][:], AF.Copy, scale=A.dtot[h][:, ci:ci + 1])
                A.stt(S[h][:], S[h][:], A.dtot[h][:, ci:ci + 1], A.pkv[h % 2][cp_][:], ALU.mult, ALU.add)
            if want_out:
                A.mm(A.po[:, hs], A.sTm[h][:], vi_t[:, hs], start=True, stop=False)
                A.mm(A.po[:, hs], q0, A.Shat[h][0][:], start=False, stop=False)
                A.mm(A.po[:, hs], q1, A.Shat[h][1][:], start=False, stop=True)

    def build(self):
        nc = self.nc
        A = self
        for k, shp in IN_SHAPES.items():
            if A.dbg is not None and A.dbg != "full1" and k.startswith("w_exp"):
                continue
            A.din(k, shp)
        for k, shp in CONST_SHAPES.items():
            A.din(k, shp)
        out = nc.dram_tensor("out", [SPC, L, DM], F32, kind="ExternalOutput").ap()
        A.D["out"] = out
        if A.dbg is not None:
            A.D["dbg"] = nc.dram_tensor("dbg", [128, 16384], F32, kind="ExternalOutput").ap()
        h2s = nc.dram_tensor("xn2s", [SPC, 16, 128, DM], BF16, kind="Internal").ap()
        D = A.D
        with ExitStack() as st:
            P = Prog(nc, st)
            A.P = P
            sb, ps = P.sb, P.ps
            A.pj0 = ps("pj0", [128, 512], F32)
            A.pj1 = ps("pj1", [128, 512], F32)
            A.pX = ps("pX", [128, 512], F32)
            A.po = ps("po", [128, 512], F32)
            A.ptr = ps("ptr", [128, 1024], BF16)
            bS = ps("bS", [128, 512], F32)
            bKV = ps("bKV", [128, 512], F32)
            b7 = ps("b7", [128, 512], F32)
            A.bS = bS
            A.bKV = bKV
            A.psT = [bS[:, h * 128:(h + 1) * 128] for h in range(4)]
            A.pkv = [[bKV[:, (h * 2 + c) * 128:(h * 2 + c + 1) * 128] for c in range(2)] for h in range(2)]
            A.pY = b7[:, 0:256]
            A.ptk = [b7[:, 256 + i * 64:256 + (i + 1) * 64].bitcast(BF16) for i in range(2)]

            A.ident_b = sb("ident_b", [128, 128], BF16)
            A.ident_f = sb("ident_f", [128, 128], F32)
            A.maskF = sb("maskF", [128, 128], F32)
            A.maskB = sb("maskB", [128, 128], F32)
            A.reset = sb("reset", [128, 512], F32)
            A.bones = sb("bones", [128, 128], BF16)
            A.perm = sb("perm", [128, 128], BF16)
            A.rot64 = sb("rot64", [128, 128], F32)
            A.ones_b = sb("ones_b", [128, 128], BF16)
            A.modT = sb("modT", [128, 48, 4], F32)
            A.sc1T = sb("sc1T", [128, 8, 3], F32)
            A.sc2T = sb("sc2T", [128, 8, 3], F32)
            A.lb = sb("lb", [128, 2, 4], F32)
            A.oml = sb("oml", [128, 2, 4], F32)
            A.noml = sb("noml", [128, 2, 4], F32)
            A.hgn_bc = sb("hgn_bc", [128, 512], F32)
            A.qng = sb("qng", [128, 1], F32)
            A.kng = sb("kng", [128, 1], F32)
            A.scT = sb("scT", [128, 3, 8], F32)
            A.wk = [sb("wk%d" % i, [128, 8, 128], BF16) for i in range(2)]
            A.wv = sb("wv", [128, 8, 128], BF16)
            A.wr = sb("wr", [128, 8, NEXP], F32)
            A.ring_i = 0
            A.tri_b = sb("tri_b", [128, 128], BF16)
            A.iota = sb("iota", [128, 256], F32)
            A.iotap = sb("iotap", [128, 2], F32)
            A.n_ss = sb("n_ss", [128, 1], F32)
            A.n_ss2 = sb("n_ss2", [128, 1], F32)
            A.n_rs = sb("n_rs", [128, 1], F32)
            A.xn = sb("xn", [128, DM], BF16)
            A.junk = A.xn
            A.wgt = [sb("wgt%d" % s, [128, 16, NEXP], F32) for s in range(SPC)]

            cdma = lambda dst, src, **kw: P.dma("sp", dst, src, key="const", **kw)
            cdma_cast = lambda dst, src, **kw: P.dma("pool", dst, src, key="constc", **kw)

            with ExitStack() as ph:
                A.ph = ph
                A.ring = [sb("ring%d" % i, [128, 8, 512], BF16, ph) for i in range(4)]
                cdma_cast(A.tri_b[:], D["c_tri"])
                cdma(A.iota[:], D["c_iota"])
                cdma(A.iotap[:], D["c_iotap"])
                cdma_cast(A.ident_b[:], D["c_ident"])
                cdma(A.ident_f[:], D["c_ident"])
                cdma(A.maskF[:], D["c_maskF"])
                cdma(A.maskB[:], D["c_maskB"])
                cdma(A.reset[:], D["c_reset"])
                cdma_cast(A.bones[:], D["c_bones"])
                cdma_cast(A.perm[:], D["c_perm"])
                cdma(A.rot64[:], D["c_rot64"])
                A.memset(A.ones_b[:], 1.0)
                cdma(A.hgn_bc[:], D["hg_norm_g"].partition_broadcast(128))
                for half in range(2):
                    cdma(A.qng[half * 64:(half + 1) * 64, :], D["q_norm_g"].rearrange("(p o) -> p o", o=1))
                    cdma(A.kng[half * 64:(half + 1) * 64, :], D["k_norm_g"].rearrange("(p o) -> p o", o=1))
                w_in3 = D["w_in"].rearrange("(kc p) j -> p kc j", p=128)
                for kvh in range(2):
                    for dup in range(2):
                        cdma_cast(A.wk[kvh][:, :, dup * 64:(dup + 1) * 64],
                                  w_in3[:, :, C_AK + kvh * 64:C_AK + (kvh + 1) * 64])
                cdma_cast(A.wv[:], w_in3[:, :, C_AV:C_AV + 128])
                for kc in range(8):
                    cdma(A.wr[:, kc, :], D["w_router"][kc * 128:(kc + 1) * 128, :])
                vrows = sb("vrows", [128, 128], F32, ph)
                vT = sb("vT", [128, 104], F32, ph)
                A.memset(vrows[:], 0.0)
                cdma(vrows[0:48, :], D["b_mod"].rearrange("(j p) -> j p", p=128))
                cdma(vrows[48:56, :], D["norm1_g"].rearrange("(j p) -> j p", p=128))
                cdma(vrows[56:64, :], D["norm2_g"].rearrange("(j p) -> j p", p=128))
                cdma(vrows[64:80, :], D["hg_lb_logits"].rearrange("d s (h p) -> (d s h) p", p=128))
                for s in range(SPC):
                    cdma(vrows[80 + 8 * s:88 + 8 * s, :], D["c"][s].rearrange("(j p) -> j p", p=128))
                cdma(vrows[96:104, :], D["c_ctx"].rearrange("(j p) -> j p", p=128))
                A.tr(A.pj0[:, 0:128], vrows[:], A.ident_f[:])
                A.cp(vT[:], A.pj0[:, 0:104])
                lg = vT[:, 64:80].rearrange("p (d s h) -> p d s h", d=2, s=2)
                dl = sb("dl", [128, 2, 4], F32, ph)
                A.tt(dl[:], lg[:, :, 0, :], lg[:, :, 1, :], ALU.subtract)
                A.act(A.lb[:], dl[:], AF.Sigmoid)
                A.act(A.oml[:], dl[:], AF.Sigmoid, scale=-1.0)
                A.ts(A.noml[:], A.oml[:], -1.0, None, ALU.mult)
                cT = vT[:, 80:104].rearrange("p (v k) -> p v k", v=3)
                scT = A.scT
                scb = sb("scb", [128, 8, 4], BF16, ph)
                A.act(scT[:], cT, AF.Silu)
                A.memset(scb[:], 0.0)
                for v in range(3):
                    A.cp(scb[:, :, v], scT[:, v, :])
                bmT = vT[:, 0:48].rearrange("p (a o) -> p a o", o=1)
                n1g = vT[:, 48:56].rearrange("p (a o) -> p a o", o=1)
                n2g = vT[:, 56:64].rearrange("p (a o) -> p a o", o=1)
                wmod3 = D["w_mod"].rearrange("(kc p) j -> p kc j", p=128)
                for g in range(12):
                    slot = A.ring_load(wmod3[:, :, g * 512:(g + 1) * 512])
                    for jl in range(4):
                        for kc in range(8):
                            A.mm(A.pj0[:, jl * 4:(jl + 1) * 4], slot[:, kc, jl * 128:(jl + 1) * 128], scb[:, kc, :],
                                 start=(kc == 0), stop=(kc == 7))
                    A.cp(A.modT[:, g * 4:(g + 1) * 4, :].rearrange("p a b -> p (a b)"), A.pj0[:, 0:16])
                A.tt(A.modT[:], A.modT[:], bmT.to_broadcast([128, 48, 4]), ALU.add)
                A.stt(A.sc1T[:], A.modT[:, 8:16, 0:3], 1.0, n1g.to_broadcast([128, 8, 3]), ALU.add, ALU.mult)
                A.stt(A.sc2T[:], A.modT[:, 32:40, 0:3], 1.0, n2g.to_broadcast([128, 8, 3]), ALU.add, ALU.mult)
                if A.dbg == "p0":
                    A.dump("modT", A.modT[:].rearrange("p a b -> p (a b)"), 192)
                    A.dump("sc1T", A.sc1T[:].rearrange("p a b -> p (a b)"), 24)
                    A.dump("lb", A.lb[:].rearrange("p a b -> p (a b)"), 8)
                P.flush()
            if A.dbg == "p0":
                return nc

            for s in range(A.nsamp):
                A.token_mix(s, h2s)
                if A.dbg is not None and A.dbg.startswith("tm"):
                    return nc
                A.moe(s, h2s)
                if A.dbg == "full1":
                    return nc
        return nc

    def token_mix(self, s, h2s):
        A = self
        P = A.P
        D = A.D
        with ExitStack() as ph:
            A.ph = ph
            sb = lambda n, shp, dt: P.sb(n, shp, dt, ph)
            A.ring = [sb("ring%d" % i, [128, 8, 512], BF16) for i in range(4)]
            xts = [sb("xt%d" % i, [128, DM], F32) for i in range(2)]
            hT = [sb("hT%d" % k, [128, BLK], BF16) for k in range(8)]
            vi = [sb("vi%d" % i, [128, 512], BF16) for i in range(4)]
            ytmp = sb("ytmp", [128, DM], F32)
            x1t = sb("x1t", [128, DM], F32)
            affT = sb("affT", [NEXP, L], F32)
            tmpf = sb("tmpf", [128, BLK], F32)
            numt = sb("numt", [128, BLK], F32)
            osum = sb("osum", [128, 512], F32)
            u1 = sb("u1", [128, 512], F32)
            A.g_sg = x1t[:, 0:BLK]
            A.g_key = x1t[:, BLK:2 * BLK]
            A.g_eP = ytmp[:, 0:BLK]
            A.g_eM = ytmp[:, BLK:2 * BLK]
            A.g_lf = tmpf[:]
            A.g_cum = numt[:]
            g1bc = sb("g1bc", [128, DM], F32)
            A.kt_ = [sb("kt_%d" % h, [128, BLK], BF16) for h in range(4)]
            A.qm0 = [sb("qm0%d" % h, [128, BLK], BF16) for h in range(4)]
            A.qm1 = [sb("qm1%d" % h, [128, BLK], BF16) for h in range(4)]
            A.dtot = [sb("dtot%d" % h, [128, 8], F32) for h in range(4)]
            A.sTm = [sb("sTm%d" % h, [128, 128], BF16) for h in range(4)]
            A.ktT0 = [sb("ktT0%d" % h, [128, 128], BF16) for h in range(4)]
            A.ktT1 = [sb("ktT1%d" % h, [128, 128], BF16) for h in range(4)]
            A.Shat = [[sb("Shat%d%d" % (h, c), [128, 128], BF16) for c in range(2)] for h in range(4)]
            Sf = [sb("Sf%d" % h, [128, 128], F32) for h in range(4)]
            Sb = [sb("Sb%d" % h, [128, 128], F32) for h in range(4)]
            of = [sb("of%d" % i, [128, 512], BF16) for i in range(16)]
            kdup = [sb("kdup%d" % i, [128, NKEY], BF16) for i in range(2)]
            vext = [sb("vext%d" % i, [128, 2, 192], BF16) for i in range(NKEY // 128)]
            A.ropeC = sb("ropeC", [128, BLK], F32)
            A.ropeS = sb("ropeS", [128, BLK], F32)
            A.qk_sq = sb("qk_sq", [128, BLK], BF16)
            A.qk_ms = tmpf[:]
            A.qk_rr = numt[:]
            A.qk_kn = x1t[:, 0:BLK]
            A.qk_knb = sb("qk_knb", [128, BLK], BF16)
            sgl = [sb("sgl%d" % i, [128, 512], BF16) for i in range(4)]
            ub = sb("ub", [128, 512], BF16)
            ss4 = sb("ss4", [128, 4], F32)
            ss4b = sb("ss4b", [128, 4], F32)
            rs4 = sb("rs4", [128, 4], F32)
            uT = [sb("uT%d" % k, [128, BLK], BF16) for k in range(4)]
            pT = [sb("pT%d" % i, [128, BLK], BF16) for i in range(3)]
            Dt = sb("Dt", [128, BLK], F32)
            OT = [sb("OT%d" % k, [128, BLK], BF16) for k in range(4)]
            sgA = [sb("sgA%d" % i, [128, BLK], BF16) for i in range(2)]
            mT = [sb("mT%d" % k, [128, BLK], BF16) for k in range(8)]
            h2f = [sb("h2f%d" % k, [128, 128], F32) for k in range(8)]
            lgt = sb("lgt", [128, NEXP], F32)
            lmx = sb("lmx", [128, 1], F32)
            lsm = sb("lsm", [128, 1], F32)
            aff = sb("aff", [128, 16, NEXP], F32)
            A.make_gbc(s, 2 * DM, g1bc, ph)
            if A.dbg == "tm_ca":
                A.dump("g1bc", g1bc[:], 1024)
                P.flush()
                return

            xi = [0]

            def load_x(src_ap):
                t = xts[xi[0] % 2]
                xi[0] += 1
                P.dma("sp", t[:], src_ap, key="x_" + t.name)
                return t

            for kt in range(NKEY // 128):
                A.memset(vext[kt][:, :, 64:128], 1.0)
            for h in range(4):
                A.memset(Sf[h][:], 0.0)
                A.memset(Sb[h][:], 0.0)
            A.memset(Dt[:], 1.0)
            for h in range(4):
                A.memset(A.qm0[h][:], 0.0)
                A.memset(A.qm1[h][:], 0.0)
                A.memset(A.ktT0[h][:], 0.0)
                A.memset(A.ktT1[h][:], 0.0)

            if A.dbg == "tm_c0":
                A.dump("g1bc", g1bc[:], 1024)
                P.flush()
                return
            for tt in range(2):
                xt = load_x(D["ctx"][s, tt * 128:(tt + 1) * 128, :])
                A.norm_to_hT(xt, 2, hT, tt * 128, A.sc1T, 0)
            if A.dbg == "tm_c1":
                A.dump("xt", xts[1][:, 0:256], 256)
                A.dump("hT0", hT[0][:, 0:256], 256)
                P.flush()
                return
            n = CTX
            w_ff = A.win_group(C_FF)
            w_fb = A.win_group(C_FB)
            w_i = A.win_group(C_I)
            for tt in range(2):
                A.proj_tm(A.pX[:], hT, tt * 128, w_i, 0, 512)
                A.cp(vi[tt][:], A.pX[:], eng="act")
            for h in range(4):
                A.proj_fm(A.pj0, w_ff, h * 128, hT, n)
                A.gates(0, h, A.pj0, None, n)
            if A.dbg == "tm_c2":
                A.dump("kt0", A.kt_[0][:, 0:256], 256)
                A.dump("vi0", vi[0][:], 512)
                P.flush()
                return
            for tt in range(2):
                A.hgrn_tile(0, tt, vi[tt], Sf, False)
            if A.dbg == "tm_c3":
                A.dump("Sf0", Sf[0][:], 128)
                P.flush()
                return
            for h in range(4):
                A.proj_fm(A.pj0, w_fb, h * 128, hT, n)
                A.gates(1, h, A.pj0, None, n)
            for tt in (1, 0):
                A.hgrn_tile(1, tt, vi[tt], Sb, False)
            for kvh in range(2):
                A.proj_fm(A.pj0, A.wk[kvh], 0, hT, n)
                A.qknorm(A.pj0, n, A.kng, kdup[kvh][:, 0:n], None)
            for tt in range(2):
                A.proj_tm(A.pX[:, 0:128], hT, tt * 128, A.wv, 0, 128)
                for kvh in range(2):
                    A.cp(vext[tt][:, kvh, 0:64], A.pX[:, kvh * 64:(kvh + 1) * 64], eng="act")
                    A.cp(vext[tt][:, kvh, 128:192], A.pX[:, kvh * 64:(kvh + 1) * 64], eng="dve")
            if A.dbg == "tm_ctx":
                A.dump("Sf0", Sf[0][:], 128)
                A.dump("Sb3", Sb[3][:], 128)
                A.dump("kdup1c", kdup[1][:, 0:256], 256)
                A.dump("vext1", vext[1][:].rearrange("p a b -> p (a b)"), 384)
                A.dump("hT0", hT[0][:, 0:256], 256)
                P.flush()
                return

            for b in range(NBLK):
                tok0 = b * BLK
                for tt in range(4):
                    xt = load_x(D["x"][s, tok0 + tt * 128:tok0 + (tt + 1) * 128, :])
                    A.norm_to_hT(xt, s, hT, tt * 128, A.sc1T, 0)
                P.dma("sp", A.ropeC[:], D["c_ropeC"][:, tok0:tok0 + BLK], key="ropeC")
                P.dma("sp", A.ropeS[:], D["c_ropeS"][:, tok0:tok0 + BLK], key="ropeS")
                w_ff = A.win_group(C_FF)
                w_q = A.win_group(C_Q)
                w_i = A.win_group(C_I)
                for h in range(4):
                    A.proj_fm(A.pj0, w_ff, h * 128, hT, BLK)
                    A.proj_fm(A.pj1, w_q, h * 128, hT, BLK)
                    A.gates(0, h, A.pj0, A.pj1, BLK)
                for tt in range(4):
                    A.proj_tm(A.pX[:], hT, tt * 128, w_i, 0, 512)
                    A.cp(vi[tt][:], A.pX[:], eng="act")
                def kstep(kvh):
                    A.proj_fm(A.pj0, A.wk[kvh], 0, hT, BLK)
                    A.qknorm(A.pj0, BLK, A.kng, kdup[kvh][:, CTX + tok0:CTX + tok0 + BLK], 0)

                def vstep(tts):
                    for t_ in tts:
                        kt = 2 + b * 4 + t_
                        A.proj_tm(A.pX[:, 0:128], hT, t_ * 128, A.wv, 0, 128)
                        for kvh in range(2):
                            A.cp(vext[kt][:, kvh, 0:64], A.pX[:, kvh * 64:(kvh + 1) * 64], eng="act")
                            A.cp(vext[kt][:, kvh, 128:192], A.pX[:, kvh * 64:(kvh + 1) * 64], eng="dve")

                side = [lambda: kstep(0), lambda: kstep(1), lambda: vstep((0, 1)), lambda: vstep((2, 3))]
                for tt in range(4):
                    A.hgrn_tile(0, tt, vi[tt], Sf, True)
                    A.cp(of[b * 4 + tt][:], A.po[:], eng="act")
                    side[tt]()
                if A.dbg == "tm_f0" and b == 0:
                    A.dump("of0", of[0][:], 512)
                    A.dump("of3", of[3][:], 512)
                    A.dump("kdup0", kdup[0][:, CTX:CTX + 512], 512)
                    A.dump("kt0", A.kt_[0][:], 512)
                    A.dump("vi0", vi[0][:], 512)
                    P.flush()
                    return
            if A.dbg == "tm_f":
                A.dump("of15", of[15][:], 512)
                A.dump("Sf1", Sf[1][:], 128)
                P.flush()
                return

            w_in3 = D["w_in"].rearrange("(kc p) j -> p kc j", p=128)
            for b in range(NBLK - 1, -1, -1):
                tok0 = b * BLK
                for tt in range(4):
                    xt = load_x(D["x"][s, tok0 + tt * 128:tok0 + (tt + 1) * 128, :])
                    A.norm_to_hT(xt, s, hT, tt * 128, A.sc1T, 0)
                P.dma("sp", A.ropeC[:], D["c_ropeC"][:, tok0:tok0 + BLK], key="ropeC")
                P.dma("sp", A.ropeS[:], D["c_ropeS"][:, tok0:tok0 + BLK], key="ropeS")
                w_fb = A.win_group(C_FB)
                w_q = A.win_group(C_Q)
                w_i = A.win_group(C_I)
                for h in range(4):
                    A.proj_fm(A.pj0, w_fb, h * 128, hT, BLK)
                    A.proj_fm(A.pj1, w_q, h * 128, hT, BLK)
                    A.gates(1, h, A.pj0, A.pj1, BLK)
                w_g = A.win_group(C_G)
                for tt in range(4):
                    A.proj_tm(A.pX[:], hT, tt * 128, w_i, 0, 512)
                    A.cp(vi[tt][:], A.pX[:], eng="act")
                    A.proj_tm(A.pj0[:], hT, tt * 128, w_g, 0, 512)
                    A.act(sgl[tt][:], A.pj0[:], AF.Silu)
                w_aq = A.win_group(C_AQ)

                def qstep(qc):
                    A.memset(mT[qc][64:128, :], 0.0)
                    A.memset(mT[4 + qc][0:64, :], 0.0)
                    A.proj_fm(A.pj0, w_aq, qc * 128, hT, BLK)
                    A.qknorm(A.pj0, BLK, A.qng, (mT[qc], mT[4 + qc]), 0)

                for qi, tt in enumerate((3, 2, 1, 0)):
                    A.hgrn_tile(1, tt, vi[tt], Sb, True)
                    gt = b * 4 + tt
                    A.tt(osum[:], A.po[:], of[gt][:], ALU.add)
                    for h in range(4):
                        A.act(A.junk[:, h * 128:(h + 1) * 128], osum[:, h * 128:(h + 1) * 128], AF.Square,
                              accum_out=ss4[:, h:h + 1])
                    A.rstd(rs4[:], ss4[:], 1.0 / 128, ss4b[:])
                    for h in range(4):
                        hs = slice(h * 128, (h + 1) * 128)
                        A.stt(u1[:, hs], osum[:, hs], rs4[:, h:h + 1], A.hgn_bc[:, hs], ALU.mult, ALU.mult)
                    A.tt(ub[:], u1[:], sgl[tt][:], ALU.mult)
                    for kc in range(4):
                        A.tr(A.ptr[:, kc * 128:(kc + 1) * 128], ub[:, kc * 128:(kc + 1) * 128], A.ident_b[:])
                    for kc in range(4):
                        A.cp(uT[kc][:, tt * 128:(tt + 1) * 128], A.ptr[:, kc * 128:(kc + 1) * 128],
                             eng=("act" if kc % 2 == 0 else "dve"))
                    qstep(qi)
                if A.dbg == "tm_b3" and b == NBLK - 1:
                    A.dump("uT0", uT[0][:], 512)
                    A.dump("uT3", uT[3][:], 512)
                    P.flush()
                    return
                NKT = NKEY // 128
                seq = [(qc, hh, kt) for qc in range(4) for hh in range(2) for kt in range(NKT)]
                pSs = (A.pj0, A.pj1)
                accs = (A.pX, A.bS)
                fxs = (A.po, A.bKV)

                def emit_S(i):
                    qc_, hh_, kt_ = seq[i]
                    A.mm(pSs[i % 2][:], kdup[qc_ // 2][:, kt_ * 128:(kt_ + 1) * 128], mT[hh_ * 4 + qc_][:])

                emit_S(0)
                for i, (qc, hh, kt) in enumerate(seq):
                    kvh = qc // 2
                    g = i // NKT
                    acc = accs[g % 2]
                    if i + 1 < len(seq):
                        emit_S(i + 1)
                    pt = pT[i % 3]
                    A.act(pt[:], pSs[i % 2][:], AF.Exp, scale=0.125)
                    A.mm(acc[:], vext[kt][:, kvh, hh * 64:hh * 64 + 128], pt[:],
                         start=(kt == 0), stop=(kt == NKT - 1))
                    if kt == NKT - 1:
                        rows = slice(hh * 64, (hh + 1) * 64)
                        den = slice((1 - hh) * 64, (2 - hh) * 64)
                        fx = fxs[g % 2]
                        A.recip(Dt[den, :], acc[den, :])
                        A.mm(fx[:], A.rot64[:], Dt[:])
                        A.cp(numt[rows, :], acc[rows, :], eng="act")
                        A.tt(OT[qc][rows, :], numt[rows, :], fx[rows, :], ALU.mult)
                if A.dbg == "tm_att" and b == NBLK - 1:
                    A.dump("qn0a", mT[0][:], 512)
                    A.dump("qn0b", mT[4][:], 512)
                    A.dump("OT0", OT[0][:], 512)
                    A.dump("OT3", OT[3][:], 512)
                    P.flush()
                    return
                w_a = A.ring_load(D["w_branch_a"].rearrange("(kc p) m -> p kc m", p=128))
                w_a4 = w_a[:].rearrange("p a b -> p (a b)").rearrange("p (a b) -> p a b", b=DM)
                for half in range(2):
                    w_ga = A.win_group(C_GA + half * 512)
                    for j in range(4):
                        dmc = half * 4 + j
                        A.proj_fm(A.pj0, w_ga, j * 128, hT, BLK)
                        sg_ = sgA[dmc % 2]
                        A.act(sg_[:], A.pj0[:], AF.Sigmoid)
                        for kc in range(4):
                            A.mm(A.pj1[:], w_a4[:, kc, dmc * 128:(dmc + 1) * 128], uT[kc][:], start=(kc == 0),
                                 stop=(kc == 3))
                        A.tt(mT[dmc][:], A.pj1[:], sg_[:], ALU.mult)
                w_b = A.ring_load(D["w_branch_b"].rearrange("(kc p) m -> p kc m", p=128))
                w_b4 = w_b[:].rearrange("p a b -> p (a b)").rearrange("p (a b) -> p a b", b=DM)
                for half in range(2):
                    w_gb = A.win_group(C_GB + half * 512)
                    for j in range(4):
                        dmc = half * 4 + j
                        A.proj_fm(A.pj0, w_gb, j * 128, hT, BLK)
                        sg_ = sgA[dmc % 2]
                        A.act(sg_[:], A.pj0[:], AF.Sigmoid)
                        for kc in range(4):
                            A.mm(A.pj1[:], w_b4[:, kc, dmc * 128:(dmc + 1) * 128], OT[kc][:], start=(kc == 0),
                                 stop=(kc == 3))
                        A.tt(tmpf[:], A.pj1[:], sg_[:], ALU.mult)
                        A.tt(mT[dmc][:], tmpf[:], mT[dmc][:], ALU.add)
                w_o = [A.ring_load(D["w_out"].rearrange("(kc p) m -> p kc m", p=128)[:, :, hf * 512:(hf + 1) * 512])
                       for hf in range(2)]
                for tt in range(4):
                    gt = b * 4 + tt
                    xt = load_x(D["x"][s, tok0 + tt * 128:tok0 + (tt + 1) * 128, :])
                    for hf in range(2):
                        pyo = A.pj0 if hf == 0 else A.pj1
                        for kc in range(8):
                            A.mm(pyo[:], mT[kc][:, tt * 128:(tt + 1) * 128], w_o[hf][:, kc, :], start=(kc == 0),
                                 stop=(kc == 7))
                        A.tt(ytmp[:, hf * 512:(hf + 1) * 512], pyo[:], g1bc[:, hf * 512:(hf + 1) * 512], ALU.mult)
                    A.tt(x1t[:], ytmp[:], xt[:], ALU.add)
                    P.dma("sp", D["out"][s, tok0 + tt * 128:tok0 + (tt + 1) * 128, :], x1t[:], key="x1st")
                    A.act(A.junk[:, 0:DM], x1t[:], AF.Square, accum_out=A.n_ss[:])
                    A.rstd(A.n_rs[:], A.n_ss[:], 1.0 / DM, A.n_ss2[:])
                    A.ts(ytmp[:], x1t[:], A.n_rs[:, 0:1], None, ALU.mult)
                    A.cp(A.xn[:], ytmp[:])
                    P.dma("sp", h2s[s, gt], A.xn[:], key="xn2st")
                    for kc in range(8):
                        pq_ = A.pj0 if kc < 4 else A.pj1
                        A.tr(pq_[:, (kc % 4) * 128:(kc % 4 + 1) * 128], ytmp[:, kc * 128:(kc + 1) * 128], A.ident_f[:])
                    for kc in range(8):
                        pq_ = A.pj0 if kc < 4 else A.pj1
                        src = pq_[:, (kc % 4) * 128:(kc % 4 + 1) * 128]
                        A.act(h2f[kc][:], src, AF.Identity, scale=A.sc2T[:, kc, s:s + 1], bias=A.modT[:, 24 + kc, s:s + 1])
                    for kc in range(8):
                        A.mm(A.pY[:, 0:NEXP], h2f[kc][:], A.wr[:, kc, :], start=(kc == 0), stop=(kc == 7))
                    A.P.op("dve", lambda e: e.reduce_max(out=lmx[:], in_=A.pY[:, 0:NEXP], axis=AX.X),
                           outs=[lmx[:]], ins=[A.pY[:, 0:NEXP]])
                    A.ts(lmx[:], lmx[:], -1.0, None, ALU.mult)
                    A.act(lgt[:], A.pY[:, 0:NEXP], AF.Exp, bias=lmx[:, 0:1], accum_out=lsm[:])
                    A.recip(lsm[:], lsm[:])
                    A.ts(aff[:, gt, :], lgt[:], lsm[:, 0:1], None, ALU.mult)
                if A.dbg == "tm_b3full" and b == NBLK - 1:
                    A.dump("x1", x1t[:], 1024)
                    A.dump("mT0", mT[0][:], 512)
                    A.dump("aff", aff[:, 15, :], 16)
                    P.flush()
                    return

            for gt in range(16):
                A.tr(A.pY[0:NEXP, 128:256], aff[:, gt, :], A.ident_f[:])
                A.cp(affT[:, gt * 128:(gt + 1) * 128], A.pY[0:NEXP, 128:256])
            lo = sb("lo", [NEXP, 1], F32)
            mid = sb("mid", [NEXP, 1], F32)
            cnt = sb("cnt", [NEXP, 1], F32)
            ge = sb("ge", [NEXP, 1], F32)
            cj = kdup[0][0:NEXP, 0:L]
            A.memset(lo[:], 0.0)
            for k in range(1, 29):
                wk_ = 2.0 ** (-k)
                A.ts(mid[:], lo[:], wk_, None, ALU.add)
                A.ts(cj, affT[:], mid[:, 0:1], 0.0, ALU.is_ge, ALU.add, accum_out=cnt[:])
                A.ts(ge[:], cnt[:], float(CAP), wk_, ALU.is_ge, ALU.mult)
                A.tt(lo[:], lo[:], ge[:], ALU.add)
            dg = sb("dg", [NEXP, NEXP], F32)
            ones16 = sb("ones16", [NEXP, 128], F32)
            thr = sb("thr", [128, 1, NEXP], F32)
            A.memset(ones16[:], 1.0)
            A.ts(dg[:], A.ident_f[0:NEXP, 0:NEXP], lo[:, 0:1], None, ALU.mult)
            A.mm(A.pY[:, 0:NEXP], ones16[:], dg[:])
            A.cp(thr[:, 0, :], A.pY[:, 0:NEXP])
            msk = sb("msk", [128, 16, NEXP], F32)
            A.tt(msk[:], aff[:], thr[:].to_broadcast([128, 16, NEXP]), ALU.is_ge)
            A.tt(A.wgt[s][:], msk[:], aff[:], ALU.mult)
            if A.dbg == "tm_all":
                A.dump("wgt", A.wgt[s][:].rearrange("p a b -> p (a b)"), 256)
                A.dump("aff", aff[:].rearrange("p a b -> p (a b)"), 256)
            P.flush()

    def moe(self, s, xn2s):
        A = self
        P = A.P
        D = A.D
        NT = L // 128
        with ExitStack() as ph:
            A.ph = ph
            sb = lambda n, shp, dt: P.sb(n, shp, dt, ph)
            ering = [sb("ering%d" % i, [128, 8, DM], BF16) for i in range(3)]
            acc = [sb("acc%d" % i, [128, DM], F32) for i in range(NT)]
            xq = [sb("xq%d" % i, [128, 4, DM], BF16) for i in range(4)]
            Sel = [sb("Sel%d" % i, [128, CAP], BF16) for i in range(NT)]
            SelT = [sb("SelT%d" % i, [128, 1024], BF16) for i in range(2)]
            hid = [sb("hid%d" % i, [128, CAP], BF16) for i in range(8)]
            xeT = [sb("xeT%d" % i, [128, CAP], BF16) for i in range(8)]
            yw = [sb("yw%d" % i, [128, DM], BF16) for i in range(2)]
            g2bc = sb("g2bc", [128, DM], F32)
            fing_bc = sb("fing_bc", [128, DM], F32)
            mf = sb("mf", [128, NT, NEXP], F32)
            mb = sb("mb", [128, NT, NEXP], BF16)
            pm = sb("pm", [128, NT, NEXP], F32)
            pmT = sb("pmT", [NEXP, L], BF16)
            rowt = sb("rowt", [NEXP, 512], BF16)
            ones16 = sb("ones16b", [NEXP, 128], BF16)
            w2 = sb("w2", [128, NT, NEXP, 2], BF16)
            wtmp = pm
            x1r = SelT[0][:].bitcast(F32)
            g4 = sb("g4", [128, 4], F32)
            gc = sb("gc", [128, 2], F32)
            ptmp = sb("ptmp", [128, NEXP], F32)
            P.dma("sp", fing_bc[:], D["final_norm_g"].partition_broadcast(128), key="fing")
            A.make_gbc(s, 5 * DM, g2bc, ph, slot_ap=ering[0][:, :, 0:512], asb=ering[1][:, :, 0:128])
            for q in range(4):
                P.dma("sp", xq[q][:], xn2s[s, q * 4:(q + 1) * 4].rearrange("t p d -> p t d"), key="xq%d" % q)
            A.memset(ones16[:], 1.0)
            wg_ = A.wgt[s]
            A.ts(mf[:], wg_[:], 0.0, None, ALU.is_gt)
            A.cp(mb[:], mf[:])
            A.cp(w2[:, :, :, 0], wg_[:])
            A.tt(wtmp[:], wg_[:], w2[:, :, :, 0], ALU.subtract)
            A.cp(w2[:, :, :, 1], wtmp[:])
            for tt in range(NT):
                A.mm(A.pY[:, 0:NEXP], A.tri_b[:], mb[:, tt, :], start=True, stop=(tt == 0))
                for t2 in range(tt):
                    A.mm(A.pY[:, 0:NEXP], A.ones_b[:], mb[:, t2, :], start=False, stop=(t2 == tt - 1))
                A.tt(ptmp[:], A.pY[:, 0:NEXP], mf[:, tt, :], ALU.mult)
                A.ts(pm[:, tt, :], ptmp[:], -1.0, None, ALU.add)
                A.tr(A.pY[0:NEXP, 128:256], pm[:, tt, :], A.ident_f[:])
                A.cp(pmT[:, tt * 128:(tt + 1) * 128], A.pY[0:NEXP, 128:256])
            eri = [0]

            def eload(src_ap):
                i = eri[0] % len(ering)
                eri[0] += 1
                P.dma("pool", ering[i][:], src_ap.rearrange("(kc p) f -> p kc f", p=128), key="ering%d" % i)
                return ering[i]

            pgs = (A.pX, A.po)
            gi = 0
            for e in range(NEXP):
                wg = eload(D["w_exp_gate"][e])
                wu = eload(D["w_exp_up"][e])
                for tt in range(NT):
                    A.ts(Sel[tt][:], A.iota[:], pm[:, tt, e:e + 1], None, ALU.is_equal)
                for kc in range(8):
                    pg = pgs[gi % 2]
                    gi += 1
                    for tt in range(NT):
                        A.mm(pg[:, 0:CAP], xq[tt // 4][:, tt % 4, kc * 128:(kc + 1) * 128], Sel[tt][:],
                             start=(tt == 0), stop=(tt == NT - 1))
                    A.act(xeT[kc][:], pg[:, 0:CAP], AF.Identity, scale=A.sc2T[:, kc, s:s + 1],
                          bias=A.modT[:, 24 + kc, s:s + 1])
                for ct in range(2):
                    for tt in range(NT):
                        A.mm(A.pY[:, ct * 2:(ct + 1) * 2], Sel[tt][:, ct * 128:(ct + 1) * 128], w2[:, tt, e, :],
                             start=(tt == 0), stop=(tt == NT - 1))
                A.cp(g4[:], A.pY[:, 0:4])
                for ct in range(2):
                    A.tt(gc[:, ct:ct + 1], g4[:, 2 * ct:2 * ct + 1], g4[:, 2 * ct + 1:2 * ct + 2], ALU.add)
                for fc in range(8):
                    for kc in range(8):
                        A.mm(A.pj0[:, 0:CAP], wg[:, kc, fc * 128:(fc + 1) * 128], xeT[kc][:], start=(kc == 0), stop=(kc == 7))
                    for kc in range(8):
                        A.mm(A.pj1[:, 0:CAP], wu[:, kc, fc * 128:(fc + 1) * 128], xeT[kc][:], start=(kc == 0), stop=(kc == 7))
                    A.act(hid[fc][:], A.pj0[:, 0:CAP], AF.Silu)
                    A.tt(hid[fc][:], hid[fc][:], A.pj1[:, 0:CAP], ALU.mult)
                wd = eload(D["w_exp_down"][e])
                for ct in range(2):
                    for hf in range(2):
                        py = pgs[gi % 2]
                        gi += 1
                        for fc in range(8):
                            A.mm(py[:], hid[fc][:, ct * 128:(ct + 1) * 128], wd[:, fc, hf * 512:(hf + 1) * 512],
                                 start=(fc == 0), stop=(fc == 7))
                        A.ts(yw[ct][:, hf * 512:(hf + 1) * 512], py[:], gc[:, ct:ct + 1], None, ALU.mult)
                for th in range(2):
                    for q in range(2):
                        blk = th * 2 + q
                        A.ts(rowt[:], pmT[:, blk * 512:(blk + 1) * 512], A.ident_f[0:NEXP, e:e + 1], None, ALU.mult)
                        pb = A.bS if q == 0 else A.bKV
                        A.mm(pb[:], ones16[:], rowt[:])
                        for ct in range(2):
                            A.ts(SelT[ct][:, q * 512:(q + 1) * 512], pb[:], A.iotap[:, ct:ct + 1], None, ALU.is_equal)
                    for tl in range(8):
                        gt = th * 8 + tl
                        for hf in range(2):
                            py = pgs[gi % 2]
                            gi += 1
                            for ct in range(2):
                                A.mm(py[:], SelT[ct][:, tl * 128:(tl + 1) * 128], yw[ct][:, hf * 512:(hf + 1) * 512],
                                     start=(ct == 0), stop=(ct == 1))
                            dst = acc[gt][:, hf * 512:(hf + 1) * 512]
                            if e == 0:
                                A.cp(dst, py[:], eng="act")
                            else:
                                A.tt(dst, py[:], dst, ALU.add)
            for gt in range(NT):
                tok = gt * 128
                fin = acc[gt]
                A.tt(fin[:], fin[:], g2bc[:], ALU.mult)
                for hf in range(2):
                    P.dma("sp", x1r, D["out"][s, tok:tok + 128, hf * 512:(hf + 1) * 512], key="x1ld")
                    A.tt(fin[:, hf * 512:(hf + 1) * 512], fin[:, hf * 512:(hf + 1) * 512], x1r, ALU.add)
                A.act(A.junk[:, 0:DM], fin[:], AF.Square, accum_out=A.n_ss[:])
                A.rstd(A.n_rs[:], A.n_ss[:], 1.0 / DM, A.n_ss2[:])
                A.stt(fin[:], fin[:], A.n_rs[:, 0:1], fing_bc[:], ALU.mult, ALU.mult)
                P.dma("sp", D["out"][s, tok:tok + 128, :], fin[:], key="ost%d" % gt)
            P.flush()


def _in_maps(inputs):
    consts = host_consts()
    maps = []
    f = lambda a: np.ascontiguousarray(np.asarray(a, dtype=np.float32))
    shared = {
        "c_ctx": f(inputs["c_ctx"]), "w_mod": f(inputs["w_mod"][0]), "b_mod": f(inputs["b_mod"][0]),
        "norm1_g": f(inputs["norm1_g"][0]), "norm2_g": f(inputs["norm2_g"][0]), "w_in": f(inputs["w_in"][0]),
        "hg_lb_logits": f(np.asarray(inputs["hg_lb_logits"])[:, 0:2, :]), "hg_norm_g": f(inputs["hg_norm_g"][0]),
        "q_norm_g": f(inputs["q_norm_g"][0]), "k_norm_g": f(inputs["k_norm_g"][0]),
        "w_branch_a": f(inputs["w_branch_a"][0]), "w_branch_b": f(inputs["w_branch_b"][0]),
        "w_out": f(inputs["w_out"][0]), "w_router": f(inputs["w_router"][0]),
        "w_exp_gate": f(inputs["w_exp_gate"][0]), "w_exp_up": f(inputs["w_exp_up"][0]),
        "w_exp_down": f(inputs["w_exp_down"][0]), "final_norm_g": f(inputs["final_norm_g"]),
    }
    shared.update(consts)
    x = np.asarray(inputs["x"], dtype=np.float32)
    c = np.asarray(inputs["c"], dtype=np.float32)
    ctx = np.asarray(inputs["ctx"], dtype=np.float32)
    for i in range(NCORES):
        m = dict(shared)
        m["x"] = np.ascontiguousarray(x[i * SPC:(i + 1) * SPC])
        m["c"] = np.ascontiguousarray(c[i * SPC:(i + 1) * SPC])
        m["ctx"] = np.ascontiguousarray(ctx[i * SPC:(i + 1) * SPC])
        maps.append(m)
    return maps


def kernel(**inputs):
    maps = _in_maps(inputs)
    nc = Builder().build()
    res = run_bass_kernel_spmd(nc, maps, core_ids=list(range(NCORES)))
    out = np.concatenate([np.asarray(r["out"], dtype=np.float32) for r in res.results], axis=0)
    return out
```
